# Optimizing a Trainium2 kernel written in Bass

```python
import math
import jax, jax.numpy as jnp
from jax import lax
import numpy as np

D_MODEL = 2048
BATCH = 4
SEQ = 4096
DEPTH = 2

SSM_WIDTH = D_MODEL // 4
SSM_CH = 16
SSM_GROUPS = SSM_WIDTH // SSM_CH
SSM_STATE = 64
DIFF_WIDTH = 3 * D_MODEL // 8
DIFF_V_DIM = 128
DIFF_HEADS = DIFF_WIDTH // DIFF_V_DIM
DIFF_QK_DIM = DIFF_V_DIM // 2
GDN_WIDTH = D_MODEL - SSM_WIDTH - DIFF_WIDTH
GDN_HEAD_DIM = 128
GDN_HEADS = GDN_WIDTH // GDN_HEAD_DIM
CONV_WIDTH = 4
GDN_CHUNK = 64
ROPE_THETA = 500000.0
ROPE_DIM = DIFF_QK_DIM // 4
Q_BLOCK = 128
D_FF = 5632
N_EXPERTS = 8
TOP_K = 2
D_FF_EXPERT = 7168
N_DENSE = (DEPTH + 1) // 2
N_MOE = DEPTH // 2
IN_PROJ_WIDTH = SSM_WIDTH + 3 * DIFF_WIDTH + 4 * GDN_WIDTH + 2 * GDN_HEADS
EPS = 1e-6
F32 = jnp.float32

kernel_name = 'hybrid_s5_diffattn_gdn_moe_block'


def rmsnorm(x, w):
    xf = x.astype(F32)
    return xf * lax.rsqrt(jnp.mean(xf * xf, axis=-1, keepdims=True) + EPS) * w.astype(F32)


def l2norm(t):
    return t * lax.rsqrt(jnp.sum(t * t, axis=-1, keepdims=True) + EPS)


def s5_mixer(u, a_re, a_im, log_dt, b_re, b_im, c_re, c_im, d_skip, w_glu):
    bsz, seq, _ = u.shape
    ug = u.astype(F32).reshape(bsz, seq, SSM_GROUPS, SSM_CH)
    lam = lax.complex(a_re.astype(F32), a_im.astype(F32))
    dt = jnp.exp(log_dt.astype(F32))[:, None]
    lam_bar = jnp.exp(lam * dt)
    b_mat = lax.complex(b_re.astype(F32), b_im.astype(F32))
    b_bar = ((lam_bar - 1.0) / lam)[:, :, None] * b_mat
    bu = jnp.einsum('gph,bsgh->bsgp', b_bar, ug.astype(jnp.complex64))
    lam_seq = jnp.broadcast_to(lam_bar, bu.shape)

    def combine(e1, e2):
        a1, s1 = e1
        a2, s2 = e2
        return a1 * a2, a2 * s1 + s2

    _, states = lax.associative_scan(combine, (lam_seq, bu), axis=1)
    c_mat = lax.complex(c_re.astype(F32), c_im.astype(F32))
    y = jnp.real(jnp.einsum('ghp,bsgp->bsgh', c_mat, states))
    y = y + d_skip.astype(F32).reshape(SSM_GROUPS, SSM_CH) * ug
    y = jax.nn.gelu(y.reshape(bsz, seq, SSM_WIDTH))
    return y * jax.nn.sigmoid(y @ w_glu.astype(F32))


def partial_rope(t, cos, sin):
    half = ROPE_DIM // 2
    t1, t2, rest = t[..., :half], t[..., half:ROPE_DIM], t[..., ROPE_DIM:]
    return jnp.concatenate([t1 * cos - t2 * sin, t2 * cos + t1 * sin, rest], axis=-1)


def diff_attention(q, k, v, positions, lam_q1, lam_k1, lam_q2, lam_k2, subln_w, lambda_init):
    bsz, seq = q.shape[:2]
    inv_freq = ROPE_THETA ** (-jnp.arange(0, ROPE_DIM, 2, dtype=F32) / ROPE_DIM)
    ang = positions.astype(F32)[:, :, None] * inv_freq
    cos = jnp.cos(ang)[:, :, None, None, :]
    sin = jnp.sin(ang)[:, :, None, None, :]
    q = partial_rope(q, cos, sin) * (DIFF_QK_DIM ** -0.5)
    k = partial_rope(k, cos, sin)
    lam = (jnp.exp(jnp.sum(lam_q1.astype(F32) * lam_k1.astype(F32)))
           - jnp.exp(jnp.sum(lam_q2.astype(F32) * lam_k2.astype(F32))) + lambda_init)
    n_blocks = seq // Q_BLOCK
    q_blocks = jnp.moveaxis(q.reshape(bsz, n_blocks, Q_BLOCK, DIFF_HEADS, 2, DIFF_QK_DIM), 1, 0)
    key_idx = jnp.arange(seq)

    def attend(args):
        q_blk, blk = args
        s = jnp.einsum('bqhmd,bkhmd->bhmqk', q_blk, k)
        q_idx = blk * Q_BLOCK + jnp.arange(Q_BLOCK)
        s = jnp.where(key_idx[None, :] <= q_idx[:, None], s, -jnp.inf)
        p = jax.nn.softmax(s, axis=-1)
        w = p[:, :, 0] - lam * p[:, :, 1]
        return jnp.einsum('bhqk,bkhd->bqhd', w, v)

    o = lax.map(attend, (q_blocks, jnp.arange(n_blocks)))
    o = jnp.moveaxis(o, 0, 1).reshape(bsz, seq, DIFF_HEADS, DIFF_V_DIM)
    o = rmsnorm(o, subln_w) * (1.0 - lambda_init)
    return o.reshape(bsz, seq, DIFF_WIDTH)


def causal_short_conv(t, w):
    ch = t.shape[-1]
    y = lax.conv_general_dilated(t, w.astype(t.dtype)[:, None, :], window_strides=(1,),
                                 padding=[(CONV_WIDTH - 1, 0)],
                                 dimension_numbers=('NWC', 'WIO', 'NWC'),
                                 feature_group_count=ch)
    return jax.nn.silu(y)


def gated_delta_net(q, k, v, a, b, z, a_log, dt_bias, gnorm_w):
    bsz, seq = q.shape[:2]
    n_chunks = seq // GDN_CHUNK
    q = l2norm(q) * (GDN_HEAD_DIM ** -0.5)
    k = l2norm(k)
    beta = jax.nn.sigmoid(b)
    g = -jnp.exp(a_log.astype(F32)) * jax.nn.softplus(a + dt_bias.astype(F32))

    def chunks(t):
        t = t.reshape((bsz, n_chunks, GDN_CHUNK) + t.shape[2:])
        return jnp.moveaxis(t, 3, 1)

    qc, kc, vc = chunks(q), chunks(k), chunks(v)
    bc, gc = chunks(beta), chunks(g)
    g_cum = jnp.cumsum(gc, axis=-1)
    idx = jnp.arange(GDN_CHUNK)
    incl = idx[:, None] >= idx[None, :]
    strict = idx[:, None] > idx[None, :]
    decay = jnp.exp(jnp.where(incl, g_cum[..., :, None] - g_cum[..., None, :], -jnp.inf))
    k_beta = kc * bc[..., None]
    v_beta = vc * bc[..., None]
    lower = jnp.where(strict, jnp.einsum('bhncd,bhned->bhnce', k_beta, kc) * decay, 0.0)
    eye = jnp.eye(GDN_CHUNK, dtype=F32)
    t_mat = lax.linalg.triangular_solve(eye + lower, jnp.broadcast_to(eye, lower.shape),
                                        left_side=True, lower=True, unit_diagonal=True)
    u = t_mat @ v_beta
    w = t_mat @ (k_beta * jnp.exp(g_cum)[..., None])
    qk = jnp.where(incl, jnp.einsum('bhncd,bhned->bhnce', qc, kc) * decay, 0.0)

    def step(state, inp):
        q_i, k_i, u_i, w_i, g_i, qk_i = inp
        v_new = u_i - w_i @ state
        o_i = (q_i * jnp.exp(g_i)[..., None]) @ state + qk_i @ v_new
        g_last = g_i[..., -1:]
        state = (state * jnp.exp(g_last)[..., None]
                 + jnp.einsum('bhcd,bhce->bhde', k_i * jnp.exp(g_last - g_i)[..., None], v_new))
        return state, o_i

    xs = tuple(jnp.moveaxis(t, 2, 0) for t in (qc, kc, u, w, g_cum, qk))
    state0 = jnp.zeros((bsz, GDN_HEADS, GDN_HEAD_DIM, GDN_HEAD_DIM), F32)
    _, o = lax.scan(step, state0, xs)
    o = jnp.moveaxis(o, 0, 2).reshape(bsz, GDN_HEADS, seq, GDN_HEAD_DIM)
    o = jnp.moveaxis(o, 1, 2)
    o = rmsnorm(o, gnorm_w) * jax.nn.silu(z)
    return o.reshape(bsz, seq, GDN_WIDTH)


def hybrid_mixer(h, positions, layer_idx, w_in, w_out,
                 ssm_a_re, ssm_a_im, ssm_log_dt, ssm_b_re, ssm_b_im, ssm_c_re, ssm_c_im, ssm_d, ssm_w_glu,
                 diff_lam_q1, diff_lam_k1, diff_lam_q2, diff_lam_k2, diff_subln,
                 gdn_conv, gdn_a_log, gdn_dt_bias, gdn_norm):
    bsz, seq, _ = h.shape
    proj = (h @ w_in.astype(F32)).astype(F32)
    sizes = [SSM_WIDTH, DIFF_WIDTH, DIFF_WIDTH, DIFF_WIDTH, 3 * GDN_WIDTH, GDN_WIDTH, GDN_HEADS, GDN_HEADS]
    cuts = [int(s) for s in np.cumsum(sizes)[:-1]]
    u, dq, dk, dv, gqkv, gz, ga, gb = jnp.split(proj, cuts, axis=-1)
    y_ssm = s5_mixer(u, ssm_a_re, ssm_a_im, ssm_log_dt, ssm_b_re, ssm_b_im,
                     ssm_c_re, ssm_c_im, ssm_d, ssm_w_glu)
    lambda_init = 0.8 - 0.6 * math.exp(-0.3 * layer_idx)
    y_diff = diff_attention(dq.reshape(bsz, seq, DIFF_HEADS, 2, DIFF_QK_DIM),
                            dk.reshape(bsz, seq, DIFF_HEADS, 2, DIFF_QK_DIM),
                            dv.reshape(bsz, seq, DIFF_HEADS, DIFF_V_DIM), positions,
                            diff_lam_q1, diff_lam_k1, diff_lam_q2, diff_lam_k2, diff_subln, lambda_init)
    gq, gk, gv = jnp.split(causal_short_conv(gqkv, gdn_conv), 3, axis=-1)
    hs = (bsz, seq, GDN_HEADS, GDN_HEAD_DIM)
    y_gdn = gated_delta_net(gq.reshape(hs), gk.reshape(hs), gv.reshape(hs), ga, gb, gz.reshape(hs),
                            gdn_a_log, gdn_dt_bias, gdn_norm)
    return jnp.concatenate([y_ssm, y_diff, y_gdn], axis=-1) @ w_out.astype(F32)


def swiglu(h, w1, w3, w2):
    return (jax.nn.silu(h @ w1.astype(F32)) * (h @ w3.astype(F32))) @ w2.astype(F32)


def moe_swiglu(h, w_router, w1, w3, w2):
    bsz, seq, d = h.shape
    t = h.reshape(bsz * seq, d)
    logits = t @ w_router.astype(F32)
    top_val, top_idx = lax.top_k(logits, TOP_K)
    gates = jax.nn.softmax(top_val, axis=-1)
    combine = jnp.einsum('tk,tke->te', gates, jax.nn.one_hot(top_idx, N_EXPERTS, dtype=F32))
    out = jnp.zeros_like(t)
    for e in range(N_EXPERTS):
        out = out + combine[:, e:e + 1] * swiglu(t, w1[e], w3[e], w2[e])
    return out.reshape(bsz, seq, d)


def setup_inputs(seed: int = 0) -> dict:
    key = jax.random.key(seed)
    ks = jax.random.split(key, 40)

    def nrm(k, shape, scale):
        return scale * jax.random.normal(k, shape, F32)

    x = nrm(ks[0], (BATCH, SEQ, D_MODEL), 1.0)
    c = nrm(ks[1], (BATCH, D_MODEL), 1.0)
    positions = (jax.random.randint(ks[2], (BATCH, 1), 0, 2048, dtype=jnp.int32)
                 + jnp.arange(SEQ, dtype=jnp.int32)[None, :])
    w_ada = nrm(ks[3], (DEPTH, D_MODEL, 6 * D_MODEL), 0.5 * D_MODEL ** -0.5)
    b_ada = nrm(ks[4], (DEPTH, 6 * D_MODEL), 0.01)
    norm_mix = 1.0 + nrm(ks[5], (DEPTH, D_MODEL), 0.01)
    norm_ffn = 1.0 + nrm(ks[6], (DEPTH, D_MODEL), 0.01)
    norm_final = 1.0 + nrm(ks[7], (D_MODEL,), 0.01)
    w_in = nrm(ks[8], (DEPTH, D_MODEL, IN_PROJ_WIDTH), D_MODEL ** -0.5)
    w_out = nrm(ks[9], (DEPTH, D_MODEL, D_MODEL), D_MODEL ** -0.5)
    ssm_a_re = -0.5 + nrm(ks[10], (DEPTH, SSM_GROUPS, SSM_STATE), 0.01)
    ssm_a_im = math.pi * jnp.arange(SSM_STATE, dtype=F32) + nrm(ks[11], (DEPTH, SSM_GROUPS, SSM_STATE), 0.01)
    ssm_log_dt = jax.random.uniform(ks[12], (DEPTH, SSM_GROUPS), F32, math.log(1e-3), math.log(1e-1))
    ssm_b_re = nrm(ks[13], (DEPTH, SSM_GROUPS, SSM_STATE, SSM_CH), (2 * SSM_CH) ** -0.5)
    ssm_b_im = nrm(ks[14], (DEPTH, SSM_GROUPS, SSM_STATE, SSM_CH), (2 * SSM_CH) ** -0.5)
    ssm_c_re = nrm(ks[15], (DEPTH, SSM_GROUPS, SSM_CH, SSM_STATE), 0.5)
    ssm_c_im = nrm(ks[16], (DEPTH, SSM_GROUPS, SSM_CH, SSM_STATE), 0.5)
    ssm_d = nrm(ks[17], (DEPTH, SSM_WIDTH), 1.0)
    ssm_w_glu = nrm(ks[18], (DEPTH, SSM_WIDTH, SSM_WIDTH), SSM_WIDTH ** -0.5)
    diff_lam_q1 = nrm(ks[19], (DEPTH, DIFF_QK_DIM), 0.1)
    diff_lam_k1 = nrm(ks[20], (DEPTH, DIFF_QK_DIM), 0.1)
    diff_lam_q2 = nrm(ks[21], (DEPTH, DIFF_QK_DIM), 0.1)
    diff_lam_k2 = nrm(ks[22], (DEPTH, DIFF_QK_DIM), 0.1)
    diff_subln = 1.0 + nrm(ks[23], (DEPTH, DIFF_V_DIM), 0.01)
    gdn_conv = nrm(ks[24], (DEPTH, CONV_WIDTH, 3 * GDN_WIDTH), CONV_WIDTH ** -0.5)
    gdn_a_log = jnp.log(jax.random.uniform(ks[25], (DEPTH, GDN_HEADS), F32, 1.0, 16.0))
    dt0 = jnp.exp(jax.random.uniform(ks[26], (DEPTH, GDN_HEADS), F32, math.log(1e-3), math.log(1e-1)))
    gdn_dt_bias = dt0 + jnp.log(-jnp.expm1(-dt0))
    gdn_norm = 1.0 + nrm(ks[27], (DEPTH, GDN_HEAD_DIM), 0.01)
    ffn_w1 = nrm(ks[28], (N_DENSE, D_MODEL, D_FF), D_MODEL ** -0.5)
    ffn_w3 = nrm(ks[29], (N_DENSE, D_MODEL, D_FF), D_MODEL ** -0.5)
    ffn_w2 = nrm(ks[30], (N_DENSE, D_FF, D_MODEL), D_FF ** -0.5)
    moe_router = nrm(ks[31], (N_MOE, D_MODEL, N_EXPERTS), D_MODEL ** -0.5)
    moe_w1 = nrm(ks[32], (N_MOE, N_EXPERTS, D_MODEL, D_FF_EXPERT), D_MODEL ** -0.5)
    moe_w3 = nrm(ks[33], (N_MOE, N_EXPERTS, D_MODEL, D_FF_EXPERT), D_MODEL ** -0.5)
    moe_w2 = nrm(ks[34], (N_MOE, N_EXPERTS, D_FF_EXPERT, D_MODEL), D_FF_EXPERT ** -0.5)
    return {'x': x, 'c': c, 'positions': positions, 'w_ada': w_ada, 'b_ada': b_ada,
            'norm_mix': norm_mix, 'norm_ffn': norm_ffn, 'norm_final': norm_final,
            'w_in': w_in, 'w_out': w_out,
            'ssm_a_re': ssm_a_re, 'ssm_a_im': ssm_a_im, 'ssm_log_dt': ssm_log_dt,
            'ssm_b_re': ssm_b_re, 'ssm_b_im': ssm_b_im, 'ssm_c_re': ssm_c_re, 'ssm_c_im': ssm_c_im,
            'ssm_d': ssm_d, 'ssm_w_glu': ssm_w_glu,
            'diff_lam_q1': diff_lam_q1, 'diff_lam_k1': diff_lam_k1,
            'diff_lam_q2': diff_lam_q2, 'diff_lam_k2': diff_lam_k2, 'diff_subln': diff_subln,
            'gdn_conv': gdn_conv, 'gdn_a_log': gdn_a_log, 'gdn_dt_bias': gdn_dt_bias, 'gdn_norm': gdn_norm,
            'ffn_w1': ffn_w1, 'ffn_w3': ffn_w3, 'ffn_w2': ffn_w2,
            'moe_router': moe_router, 'moe_w1': moe_w1, 'moe_w3': moe_w3, 'moe_w2': moe_w2}


def reference(x, c, positions, w_ada, b_ada, norm_mix, norm_ffn, norm_final, w_in, w_out,
              ssm_a_re, ssm_a_im, ssm_log_dt, ssm_b_re, ssm_b_im, ssm_c_re, ssm_c_im, ssm_d, ssm_w_glu,
              diff_lam_q1, diff_lam_k1, diff_lam_q2, diff_lam_k2, diff_subln,
              gdn_conv, gdn_a_log, gdn_dt_bias, gdn_norm,
              ffn_w1, ffn_w3, ffn_w2, moe_router, moe_w1, moe_w3, moe_w2):
    h_res = x.astype(F32)
    cond = jax.nn.silu(c.astype(F32))
    for l in range(DEPTH):
        mod = cond @ w_ada[l].astype(F32) + b_ada[l].astype(F32)
        shift1, scale1, gate1, shift2, scale2, gate2 = jnp.split(mod[:, None, :], 6, axis=-1)
        hn = rmsnorm(h_res, norm_mix[l]) * (1.0 + scale1) + shift1
        mix = hybrid_mixer(hn, positions, l, w_in[l], w_out[l],
                           ssm_a_re[l], ssm_a_im[l], ssm_log_dt[l], ssm_b_re[l], ssm_b_im[l],
                           ssm_c_re[l], ssm_c_im[l], ssm_d[l], ssm_w_glu[l],
                           diff_lam_q1[l], diff_lam_k1[l], diff_lam_q2[l], diff_lam_k2[l], diff_subln[l],
                           gdn_conv[l], gdn_a_log[l], gdn_dt_bias[l], gdn_norm[l])
        h_res = h_res + gate1 * mix
        hn = rmsnorm(h_res, norm_ffn[l]) * (1.0 + scale2) + shift2
        if l % 2 == 0:
            f = swiglu(hn, ffn_w1[l // 2], ffn_w3[l // 2], ffn_w2[l // 2])
        else:
            f = moe_swiglu(hn, moe_router[l // 2], moe_w1[l // 2], moe_w3[l // 2], moe_w2[l // 2])
        h_res = h_res + gate2 * f
    return rmsnorm(h_res, norm_final).astype(x.dtype)
```

```python
import math
import numpy as np
from contextlib import ExitStack
import concourse.bass as bass
import concourse.mybir as mybir
from concourse.bass_utils import run_bass_kernel_spmd

F32 = mybir.dt.float32
BF16 = mybir.dt.bfloat16
I32 = mybir.dt.int32
ALU = mybir.AluOpType
AF = mybir.ActivationFunctionType
AX = mybir.AxisListType


class Prog:
    ENGS = ("pe", "act", "dve", "pool", "sp")

    def __init__(self, nc):
        self.nc = nc
        self.ops = []
        self.stack = ExitStack()

    def sb(self, name, shape, dt):
        return self.stack.enter_context(self.nc.sbuf_tensor(name, list(shape), dt))

    def ps(self, name, shape, dt=F32):
        return self.stack.enter_context(self.nc.psum_tensor(name, list(shape), dt))

    def dram(self, name, shape, dt, kind):
        return self.nc.dram_tensor(name, list(shape), dt, kind=kind).ap()

    def op(self, eng, fn, r=(), w=(), dma=False):
        self.ops.append(dict(eng=eng, fn=fn, r=tuple(r), w=tuple(w), dma=dma))

    def dma(self, out, in_, r=(), w=(), q="sp", **kw):
        self.op(q, lambda e: e.dma_start(out=out, in_=in_, **kw), r=r, w=w, dma=True)

    def emit(self):
        nc = self.nc
        ops = self.ops
        last_w = {}
        dma_w_cnt = {}
        dma_r_cnt = {}
        readers = {}
        prev_readers = {}
        dma_readers_pending = {}
        in_dma_fill = {}
        sig_needed = set()
        waits = [[] for _ in ops]
        for i, o in enumerate(ops):
            need_ops = set()
            need_dma = {}
            def need_writer(k):
                lw = last_w.get(k)
                if lw is None:
                    return
                if lw[0] == "op":
                    need_ops.add(lw[1])
                else:
                    need_dma[("w", k)] = max(need_dma.get(("w", k), 0), lw[1])
            for k in o["r"]:
                need_writer(k)
            for k in o["w"]:
                if o["dma"] and in_dma_fill.get(k):
                    rs = prev_readers.get(k, [])
                else:
                    need_writer(k)
                    rs = readers.get(k, [])
                for j in rs:
                    need_ops.add(j)
                if dma_r_cnt.get(k, 0):
                    need_dma[("r", k)] = dma_r_cnt[k]
            need_ops.discard(i)
            o["need_ops"] = need_ops
            o["need_dma"] = need_dma
            for j in need_ops:
                sig_needed.add(j)
            for k in o["r"]:
                if o["dma"]:
                    dma_r_cnt[k] = dma_r_cnt.get(k, 0) + 1
                else:
                    readers.setdefault(k, []).append(i)
                in_dma_fill[k] = False
            for k in o["w"]:
                if o["dma"]:
                    if not in_dma_fill.get(k):
                        prev_readers[k] = readers.get(k, [])
                    dma_w_cnt[k] = dma_w_cnt.get(k, 0) + 1
                    last_w[k] = ("dma", dma_w_cnt[k])
                    in_dma_fill[k] = True
                else:
                    last_w[k] = ("op", i)
                    in_dma_fill[k] = False
                readers[k] = []
        dma_keys = set()
        for o in ops:
            if o["dma"]:
                o["incs"] = [("w", k) for k in o["w"]] + [("r", k) for k in o["r"]]
                for s in o["incs"]:
                    dma_keys.add(s)
        sems = {}
        for e in ("pe", "act", "dve", "pool", "sp"):
            sems[e] = self.stack.enter_context(nc.semaphore("s_" + e))
        for n, s in enumerate(sorted(dma_keys, key=str)):
            sems[s] = self.stack.enter_context(nc.semaphore("d%d" % n))
        self.n_sems = len(sems)
        cnt = {e: 0 for e in self.ENGS}
        sigval = {}
        for i, o in enumerate(ops):
            if i in sig_needed and not o["dma"]:
                cnt[o["eng"]] += 1
                sigval[i] = cnt[o["eng"]]
        final_dma = {}
        for o in ops:
            if o["dma"]:
                for s in o["incs"]:
                    final_dma[s] = final_dma.get(s, 0) + 16
        by_eng = {e: [] for e in self.ENGS}
        for i, o in enumerate(ops):
            by_eng[o["eng"]].append(i)
        block = self.stack.enter_context(nc.Block())
        self.n_waits = 0

        def run_engine(ename, eng):
            waited = {}
            for i in by_eng[ename]:
                o = ops[i]
                reqs = {}
                for j in o["need_ops"]:
                    pj = ops[j]
                    if pj["dma"]:
                        continue
                    s = pj["eng"]
                    reqs[s] = max(reqs.get(s, 0), sigval[j])
                for s, n in o["need_dma"].items():
                    reqs[s] = max(reqs.get(s, 0), 16 * n)
                for s, v in reqs.items():
                    if waited.get(s, 0) >= v:
                        continue
                    eng.wait_ge(sems[s], v)
                    waited[s] = v
                    self.n_waits += 1
                ins = o["fn"](eng)
                if o["dma"]:
                    assert len(o["incs"]) >= 1
                    assert len(o["incs"]) == 1, "dma with >1 tracked resource: %s" % (o["incs"],)
                    ins.then_inc(sems[o["incs"][0]], 16)
                elif i in sigval:
                    ins.then_inc(sems[ename], 1)
            if ename == "sp":
                for s, v in final_dma.items():
                    eng.wait_ge(sems[s], v)

        @block.sync
        def _(e):
            run_engine("sp", e)

        @block.tensor
        def _(e):
            run_engine("pe", e)

        @block.scalar
        def _(e):
            run_engine("act", e)

        @block.vector
        def _(e):
            run_engine("dve", e)

        @block.gpsimd
        def _(e):
            run_engine("pool", e)

    def close(self):
        self.stack.close()


def _mm(P, out, lhsT, rhs, r, w, start=True, stop=True, **kw):
    P.op("pe", lambda e: e.matmul(out, lhsT=lhsT, rhs=rhs, start=start, stop=stop, **kw), r=r, w=w)


def _tr(P, out, in_, ident, r, w):
    P.op("pe", lambda e: e.transpose(out, in_, ident), r=r, w=w)


def _act(P, out, in_, func, r, w, eng="act", **kw):
    P.op(eng, lambda e: e.activation(out=out, in_=in_, func=func, **kw), r=r, w=w)


def _ts(P, out, in0, s1, s2, op0, op1, r, w, eng="dve"):
    if s2 is None:
        P.op(eng, lambda e: e.tensor_scalar(out=out, in0=in0, scalar1=s1, scalar2=None, op0=op0), r=r, w=w)
    else:
        P.op(eng, lambda e: e.tensor_scalar(out=out, in0=in0, scalar1=s1, scalar2=s2, op0=op0, op1=op1), r=r, w=w)


def _tt(P, out, in0, in1, op, r, w, eng="dve"):
    P.op(eng, lambda e: e.tensor_tensor(out=out, in0=in0, in1=in1, op=op), r=r, w=w)


def _stt(P, out, in0, scalar, in1, op0, op1, r, w, eng="dve"):
    P.op(eng, lambda e: e.scalar_tensor_tensor(out=out, in0=in0, scalar=scalar, in1=in1, op0=op0, op1=op1), r=r, w=w)


def _cp(P, out, in_, r, w, eng="dve"):
    if eng == "act":
        P.op(eng, lambda e: e.copy(out=out, in_=in_), r=r, w=w)
    else:
        P.op(eng, lambda e: e.tensor_copy(out=out, in_=in_), r=r, w=w)


def _rcp(P, out, in_, r, w):
    P.op("dve", lambda e: e.reciprocal(out=out, in_=in_), r=r, w=w)


D = 2048
KC = 16
EPS = 1e-6


def pscal(v):
    return np.ascontiguousarray(np.asarray(v, np.float32).reshape(KC, 128).T)


def build_k0():
    nc = bass.Bass("TRN2", target_bir_lowering=False)
    P = Prog(nc)
    NCOL = 3072
    cT = P.dram("cT", [128, KC, 4], F32, "ExternalInput")
    w = P.dram("w", [D, NCOL], F32, "ExternalInput")
    bias = P.dram("bias", [4, NCOL], F32, "ExternalInput")
    out = P.dram("out", [4, NCOL], F32, "ExternalOutput")
    c_sb = P.sb("c_sb", [128, KC, 4], F32)
    b_sb = P.sb("b_sb", [4, NCOL], F32)
    o_sb = P.sb("o_sb", [4, NCOL], F32)
    wb = [P.sb("wb%d" % i, [128, KC, 512], F32) for i in range(2)]
    pp = [P.ps("pp%d" % i, [4, 512]) for i in range(2)]
    P.dma(c_sb[:], cT, w=["c"])
    P.dma(b_sb[:], bias, w=["b"])
    P.op("act", lambda e: e.activation(out=c_sb[:], in_=c_sb[:], func=AF.Silu), r=["c"], w=["c"])
    wv = w.rearrange("(kc p) n -> p kc n", p=128)
    for g in range(NCOL // 512):
        s = g % 2
        P.dma(wb[s][:], wv[:, :, g * 512:(g + 1) * 512], w=["wb%d" % s], q=("sp" if s == 0 else "act"))
        for kc in range(KC):
            P.op("pe", lambda e, kc=kc, s=s: e.matmul(pp[s][:], lhsT=c_sb[:, kc, :], rhs=wb[s][:, kc, :],
                                                       start=(kc == 0), stop=(kc == KC - 1)),
                 r=["c", "wb%d" % s], w=["pp%d" % s])
        P.op("dve", lambda e, g=g, s=s: e.tensor_tensor(out=o_sb[:, g * 512:(g + 1) * 512], in0=pp[s][:],
                                                         in1=b_sb[:, g * 512:(g + 1) * 512], op=ALU.add),
             r=["pp%d" % s, "b"], w=["o"])
    P.dma(out, o_sb[:], r=["o"])
    P.emit()
    P.close()
    return nc


def run_k0(c, w_ada, b_ada):
    nc = build_k0()
    cT = np.ascontiguousarray(c.T.reshape(KC, 128, 4).transpose(1, 0, 2))
    in_maps = []
    for i in range(8):
        l, j = divmod(i, 4)
        cols = slice(j * 3072, (j + 1) * 3072)
        in_maps.append({"cT": cT, "w": np.ascontiguousarray(w_ada[l][:, cols]),
                        "bias": np.ascontiguousarray(np.broadcast_to(b_ada[l][cols], (4, 3072)))})
    res = run_bass_kernel_spmd(nc, in_maps, core_ids=list(range(8)))
    mod = np.zeros((2, 4, 12288), np.float32)
    for i in range(8):
        l, j = divmod(i, 4)
        mod[l, :, j * 3072:(j + 1) * 3072] = res.results[i]["out"]
    return mod


def build_ka(NTOK=2048, NOUT=5900):
    nc = bass.Bass("TRN2", target_bir_lowering=False)
    P = Prog(nc)
    NB = NTOK // 128
    x = P.dram("x", [NTOK, D], F32, "ExternalInput")
    w = P.dram("w", [D, NOUT], F32, "ExternalInput")
    vecs = P.dram("vecs", [128, 3, KC], F32, "ExternalInput")
    ident = P.dram("ident", [128, 128], F32, "ExternalInput")
    out = P.dram("out", [NTOK, NOUT], F32, "ExternalOutput")
    v_sb = P.sb("v_sb", [128, 3, KC], F32)
    A_sb = P.sb("A_sb", [128, KC], F32)
    id_sb = P.sb("id_sb", [128, 128], F32)
    hnT = P.sb("hnT", [128, KC, NTOK], BF16)
    xb = [P.sb("xb%d" % i, [128, D], F32) for i in range(2)]
    junk = P.sb("junk", [128, D], F32)
    st = P.sb("st", [128, 4], F32)
    tp = [P.ps("tp%d" % i, [128, 512]) for i in range(2)]
    mp = [P.ps("mp%d" % i, [128, 512]) for i in range(2)]
    wb = [P.sb("wb%d" % i, [128, KC, 512], BF16) for i in range(2)]
    ob = [P.sb("ob%d" % i, [128, 512], F32) for i in range(2)]
    P.dma(v_sb[:], vecs, w=["v"])
    P.dma(id_sb[:], ident, w=["id"])
    P.op("dve", lambda e: e.scalar_tensor_tensor(out=A_sb[:], in0=v_sb[:, 1, :], scalar=1.0, in1=v_sb[:, 0, :],
                                                 op0=ALU.add, op1=ALU.mult), r=["v"], w=["A"])
    for b in range(NB):
        s = b % 2
        P.dma(xb[s][:], x[b * 128:(b + 1) * 128, :], w=["xb%d" % s])
        P.op("act", lambda e, s=s: e.activation(out=junk[:], in_=xb[s][:], func=AF.Square, accum_out=st[:, 0:1]),
             r=["xb%d" % s], w=["junk", "st"])
        P.op("act", lambda e: e.activation(out=st[:, 1:2], in_=st[:, 0:1], func=AF.Sqrt, scale=1.0 / D, bias=st[:, 3:4]),
             r=["st", "eps"], w=["st"])
        P.op("dve", lambda e: e.reciprocal(out=st[:, 2:3], in_=st[:, 1:2]), r=["st"], w=["st"])
        P.op("dve", lambda e, s=s: e.tensor_scalar(out=xb[s][:], in0=xb[s][:], scalar1=st[:, 2:3], scalar2=None,
                                                    op0=ALU.mult), r=["st", "xb%d" % s], w=["xb%d" % s])
        for q in range(4):
            t = (b * 4 + q) % 2
            for j in range(4):
                kc = q * 4 + j
                P.op("pe", lambda e, s=s, t=t, j=j, kc=kc: e.transpose(tp[t][:, j * 128:(j + 1) * 128],
                                                                        xb[s][:, kc * 128:(kc + 1) * 128], id_sb[:]),
                     r=["xb%d" % s, "id"], w=["tp%d" % t])
            for j in range(4):
                kc = q * 4 + j
                P.op("dve" if j % 2 == 0 else "pool" if False else "dve",
                     lambda e, t=t, j=j, kc=kc, b=b: e.tensor_scalar(
                         out=hnT[:, kc, b * 128:(b + 1) * 128], in0=tp[t][:, j * 128:(j + 1) * 128],
                         scalar1=A_sb[:, kc:kc + 1], scalar2=v_sb[:, 2, kc:kc + 1], op0=ALU.mult, op1=ALU.add),
                     r=["tp%d" % t, "A", "v"], w=["hnT%d" % b])
    wv = w.rearrange("(kc p) n -> p kc n", p=128)
    ng = (NOUT + 511) // 512
    cnt = 0
    for g in range(ng):
        c0 = g * 512
        cw = min(512, NOUT - c0)
        s = g % 2
        P.dma(wb[s][:, :, 0:cw], wv[:, :, c0:c0 + cw], w=["wb%d" % s], q="pool")
        for b in range(NB):
            m = cnt % 2
            cnt += 1
            for kc in range(KC):
                P.op("pe", lambda e, m=m, s=s, kc=kc, b=b, cw=cw: e.matmul(
                    mp[m][:, 0:cw], lhsT=hnT[:, kc, b * 128:(b + 1) * 128], rhs=wb[s][:, kc, 0:cw],
                    start=(kc == 0), stop=(kc == KC - 1)), r=["hnT%d" % b, "wb%d" % s], w=["mp%d" % m])
            P.op("act" if m == 0 else "dve",
                 (lambda e, m=m, cw=cw: e.copy(out=ob[m][:, 0:cw], in_=mp[m][:, 0:cw])) if m == 0 else
                 (lambda e, m=m, cw=cw: e.tensor_copy(out=ob[m][:, 0:cw], in_=mp[m][:, 0:cw])),
                 r=["mp%d" % m], w=["ob%d" % m])
            P.dma(out[b * 128:(b + 1) * 128, c0:c0 + cw], ob[m][:, 0:cw], r=["ob%d" % m], q=("sp" if m == 0 else "act"))
    P.ops.insert(0, dict(eng="pool", fn=lambda e: e.memset(st[:, 3:4], EPS), r=(), w=("eps",), dma=False))
    P.emit()
    P.close()
    return nc


import math

PI_SAFE = 3.141592
TWO_PI = 2.0 * math.pi


def att_consts():
    invf = np.zeros((128, 1), np.float32)
    sgn = np.ones((128, 1), np.float32)
    for p in range(128):
        d = p % 64
        if d < 16:
            j = d % 8
            invf[p, 0] = np.float32(500000.0) ** np.float32(-(2.0 * j) / 16.0)
            sgn[p, 0] = -1.0 if d < 8 else 1.0
    k = np.arange(128)[:, None]
    q = np.arange(128)[None, :]
    tri = (q >= k).astype(np.float32)
    return invf, sgn, tri


def rope_perm_index():
    perm = np.arange(128)
    for base in (0, 64):
        for d in range(8):
            perm[base + d] = base + d + 8
            perm[base + 8 + d] = base + d
    return perm


def sincos_tables(P, ang, C, S, tmpi, tmpf, keys_in, key_c, key_s, sgn_ap=None):
    def reduce_to(dst, src, shift):
        P.op("dve", lambda e: e.tensor_scalar(out=tmpi, in0=src, scalar1=shift, scalar2=1.0 / TWO_PI,
                                              op0=ALU.add, op1=ALU.mult), r=keys_in, w=["sc_tmpi"])
        P.op("dve", lambda e: e.tensor_copy(out=tmpf, in_=tmpi), r=["sc_tmpi"], w=["sc_tmpf"])
        P.op("dve", lambda e: e.scalar_tensor_tensor(out=tmpf, in0=tmpf, scalar=-TWO_PI, in1=src,
                                                     op0=ALU.mult, op1=ALU.add), r=["sc_tmpf"] + keys_in, w=["sc_tmpf"])
        P.op("dve", lambda e: e.tensor_scalar(out=tmpf, in0=tmpf, scalar1=shift, scalar2=-PI_SAFE,
                                              op0=ALU.add, op1=ALU.max), r=["sc_tmpf"], w=["sc_tmpf"])
        P.op("dve", lambda e: e.tensor_scalar(out=tmpf, in0=tmpf, scalar1=PI_SAFE, scalar2=None,
                                              op0=ALU.min), r=["sc_tmpf"], w=["sc_tmpf"])
        P.op("act", lambda e: e.activation(out=dst, in_=tmpf, func=AF.Sin), r=["sc_tmpf"], w=[dst_key[0]])
    dst_key = [key_s]
    reduce_to(S, ang, 0.0)
    if sgn_ap is not None:
        P.op("dve", lambda e: e.tensor_scalar(out=S, in0=S, scalar1=sgn_ap, scalar2=None, op0=ALU.mult),
             r=[key_s, "consts"], w=[key_s])
    dst_key[0] = key_c
    reduce_to(C, ang, math.pi / 2.0)


def build_katt(S=4096, NH=3, lambda_init=0.2):
    nc = bass.Bass("TRN2", target_bir_lowering=False)
    P = Prog(nc)
    NBLK = S // 128
    qT = P.dram("qT", [NH, 128, S], F32, "ExternalInput")
    qTp = P.dram("qTp", [NH, 128, S], F32, "ExternalInput")
    kT = P.dram("kT", [NH, 128, S], F32, "ExternalInput")
    kTp = P.dram("kTp", [NH, 128, S], F32, "ExternalInput")
    v = P.dram("v", [S, NH, 128], F32, "ExternalInput")
    pos = P.dram("pos", [128, S], I32, "ExternalInput")
    invf_d = P.dram("invf", [128, 1], F32, "ExternalInput")
    sgn_d = P.dram("sgn", [128, 1], F32, "ExternalInput")
    tri_d = P.dram("tri", [128, 128], F32, "ExternalInput")
    lamv_d = P.dram("lamv", [128, 4, 64], F32, "ExternalInput")
    subln_d = P.dram("subln", [128, 128], F32, "ExternalInput")
    y = P.dram("y", [S, NH, 128], F32, "ExternalOutput")

    invf = P.sb("invf_sb", [128, 1], F32)
    sgn = P.sb("sgn_sb", [128, 1], F32)
    tri = P.sb("tri_sb", [128, 128], BF16)
    lamv = P.sb("lamv_sb", [128, 4, 64], F32)
    subln = P.sb("subln_sb", [128, 128], F32)
    posi = P.sb("posi", [128, S], I32)
    ang = P.sb("ang", [128, S], F32)
    tmpi = P.sb("tmpi", [128, S], I32)
    tmpf = P.sb("tmpf", [128, S], F32)
    Ct = P.sb("Ct", [128, S], F32)
    St = P.sb("St", [128, S], F32)
    stg = [P.sb("stg%d" % i, [128, S], F32) for i in range(2)]
    qb = P.sb("qb", [128, S], BF16)
    kb_ = P.sb("kb", [128, S], BF16)
    vext = P.sb("vext", [128, NBLK, NH, 132], BF16)
    zer = P.sb("zer", [128, 128], BF16)
    lam = P.sb("lam", [128, 8], F32)
    lj = P.sb("lj", [128, 64], F32)
    pT = [P.sb("pT%d" % i, [128, 2, 256], BF16) for i in range(3)]
    sps = [P.ps("sps%d" % i, [128, 2, 256]) for i in range(2)]
    acc = [[P.ps("acc%d_%d" % (i, m), [128, 2, 256]) for m in range(2)] for i in range(2)]
    fin = [P.sb("fin%d" % i, [128, 8], F32) for i in range(2)]
    o0 = [P.sb("o0_%d" % i, [128, 128], F32) for i in range(2)]
    o1 = [P.sb("o1_%d" % i, [128, 128], F32) for i in range(2)]
    yb = [P.sb("yb%d" % i, [128, 128], F32) for i in range(2)]
    epsc = P.sb("epsc", [128, 1], F32)

    P.dma(invf[:], invf_d, w=["consts"])
    P.dma(sgn[:], sgn_d, w=["consts"])
    P.dma(tri[:], tri_d, w=["tri"], q="pool")
    P.dma(lamv[:], lamv_d, w=["lamv"])
    P.dma(subln[:], subln_d, w=["subln"])
    P.dma(posi[:], pos, w=["posi"])
    P.op("pool", lambda e: e.memset(zer[:], 0.0), w=["zer"])
    P.op("pool", lambda e: e.memset(epsc[:], 1e-6), w=["epsc"])
    P.op("pool", lambda e: e.memset(vext[:], 1.0), w=["vext"])
    for t in range(2):
        P.op("dve", lambda e, t=t: e.tensor_tensor(out=lj[:], in0=lamv[:, 2 * t, :], in1=lamv[:, 2 * t + 1, :], op=ALU.mult),
             r=["lamv"], w=["lj"])
        P.op("dve", lambda e, t=t: e.reduce_sum(out=lam[:, t:t + 1], in_=lj[:], axis=AX.X), r=["lj"], w=["lam"])
    P.op("act", lambda e: e.activation(out=lam[:, 2:4], in_=lam[:, 0:2], func=AF.Exp), r=["lam"], w=["lam"])
    P.op("dve", lambda e: e.tensor_tensor(out=lam[:, 4:5], in0=lam[:, 2:3], in1=lam[:, 3:4], op=ALU.subtract), r=["lam"], w=["lam"])
    P.op("dve", lambda e: e.tensor_scalar(out=lam[:, 5:6], in0=lam[:, 4:5], scalar1=float(lambda_init), scalar2=-1.0,
                                          op0=ALU.add, op1=ALU.mult), r=["lam"], w=["lam"])
    P.op("dve", lambda e: e.tensor_scalar(out=subln[:], in0=subln[:], scalar1=float(1.0 - lambda_init), scalar2=None, op0=ALU.mult),
         r=["subln"], w=["subln"])
    P.op("dve", lambda e: e.tensor_copy(out=ang[:], in_=posi[:]), r=["posi"], w=["ang"])
    P.op("dve", lambda e: e.tensor_scalar(out=ang[:], in0=ang[:], scalar1=invf[:, 0:1], scalar2=None, op0=ALU.mult),
         r=["ang", "consts"], w=["ang"])
    sincos_tables(P, ang[:], Ct[:], St[:], tmpi[:], tmpf[:], ["ang"], "Ct", "St", sgn_ap=sgn[:, 0:1])
    vv = v.rearrange("(n p) h d -> p n h d", p=128)
    for h in range(NH):
        P.dma(vext[:, :, h, 0:128], vv[:, :, h, :], w=["vext"], q="pool")

    gi = 0
    for h in range(NH):
        for (src, srcp, dst, dk) in ((qT, qTp, qb, "qb"), (kT, kTp, kb_, "kb")):
            P.dma(stg[0][:], src[h], w=["stg0"])
            P.dma(stg[1][:], srcp[h], w=["stg1"], q="act")
            P.op("dve", lambda e: e.tensor_tensor(out=stg[0][:], in0=stg[0][:], in1=Ct[:], op=ALU.mult), r=["stg0", "Ct"], w=["stg0"])
            P.op("pool", lambda e: e.tensor_tensor(out=stg[1][:], in0=stg[1][:], in1=St[:], op=ALU.mult), r=["stg1", "St"], w=["stg1"])
            P.op("dve", lambda e, dst=dst: e.tensor_tensor(out=dst[:], in0=stg[0][:], in1=stg[1][:], op=ALU.add),
                 r=["stg0", "stg1"], w=[dk])
        NG = S // 256
        for g in range(NG):
            a = gi % 2
            gi += 1
            for m in range(2):
                P.op("pe", lambda e, a=a, m=m: e.matmul(acc[a][m][:].rearrange("p a b -> p (a b)"), lhsT=zer[:], rhs=qb[:, 0:512],
                                                         start=True, stop=False), r=["zer", "qb"], w=["acc%d_%d" % (a, m)])
            nkb = 2 * g + 2
            for kb in range(nkb):
                sidx = kb % 2
                pidx = kb % 3
                diag0 = (kb == 2 * g)
                diag1 = (kb == 2 * g + 1)
                q0 = 256 * g + (128 if diag1 else 0)
                qn = 128 if diag1 else 256
                qo = 128 if diag1 else 0
                for m in range(2):
                    P.op("pe", lambda e, m=m, sidx=sidx, kb=kb, q0=q0, qn=qn, qo=qo: e.matmul(
                        sps[sidx][:, m, qo:qo + qn], lhsT=kb_[m * 64:(m + 1) * 64, kb * 128:(kb + 1) * 128],
                        rhs=qb[m * 64:(m + 1) * 64, q0:q0 + qn], start=True, stop=True),
                        r=["kb", "qb"], w=["sps%d" % sidx])
                P.op("act", lambda e, sidx=sidx, pidx=pidx, qo=qo, qn=qn: e.activation(
                    out=pT[pidx][:, :, qo:qo + qn], in_=sps[sidx][:, :, qo:qo + qn], func=AF.Exp, scale=0.125),
                    r=["sps%d" % sidx], w=["pT%d" % pidx])
                if diag0 or diag1:
                    for m in range(2):
                        P.op("pool", lambda e, m=m, pidx=pidx, qo=qo: e.tensor_tensor(
                            out=pT[pidx][:, m, qo:qo + 128], in0=pT[pidx][:, m, qo:qo + 128], in1=tri[:], op=ALU.mult),
                            r=["pT%d" % pidx, "tri"], w=["pT%d" % pidx])
                for m in range(2):
                    for qs in range(2):
                        if diag1 and qs == 0:
                            continue
                        last = (kb == 2 * g + qs)
                        P.op("pe", lambda e, a=a, m=m, qs=qs, pidx=pidx, kb=kb, h=h, last=last: e.matmul(
                            acc[a][m][:, qs, 0:129], lhsT=pT[pidx][:, m, qs * 128:(qs + 1) * 128], rhs=vext[:, kb, h, 0:129],
                            start=False, stop=last, skip_group_check=True), r=["pT%d" % pidx, "vext"], w=["acc%d_%d" % (a, m)])
            for qs in range(2):
                f = fin[qs]
                blk = 2 * g + qs
                P.op("dve", lambda e, a=a, qs=qs, f=f: e.reciprocal(out=f[:, 0:1], in_=acc[a][0][:, qs, 128:129]), r=["acc%d_0" % a], w=["fin%d" % qs])
                P.op("dve", lambda e, a=a, qs=qs, f=f: e.reciprocal(out=f[:, 1:2], in_=acc[a][1][:, qs, 128:129]), r=["acc%d_1" % a], w=["fin%d" % qs])
                P.op("dve", lambda e, f=f: e.tensor_tensor(out=f[:, 2:3], in0=f[:, 1:2], in1=lam[:, 5:6], op=ALU.mult), r=["fin%d" % qs, "lam"], w=["fin%d" % qs])
                P.op("dve", lambda e, a=a, qs=qs, f=f: e.tensor_scalar(out=o0[qs][:], in0=acc[a][0][:, qs, 0:128], scalar1=f[:, 0:1], scalar2=None, op0=ALU.mult),
                     r=["acc%d_0" % a, "fin%d" % qs], w=["o0_%d" % qs])
                P.op("dve", lambda e, a=a, qs=qs, f=f: e.scalar_tensor_tensor(out=o1[qs][:], in0=acc[a][1][:, qs, 0:128], scalar=f[:, 2:3], in1=o0[qs][:],
                                                                             op0=ALU.mult, op1=ALU.add),
                     r=["acc%d_1" % a, "fin%d" % qs, "o0_%d" % qs], w=["o1_%d" % qs])
                P.op("act", lambda e, qs=qs, f=f: e.activation(out=o0[qs][:], in_=o1[qs][:], func=AF.Square, accum_out=f[:, 3:4]),
                     r=["o1_%d" % qs], w=["o0_%d" % qs, "fin%d" % qs])
                P.op("act", lambda e, f=f: e.activation(out=f[:, 4:5], in_=f[:, 3:4], func=AF.Sqrt, scale=1.0 / 128.0, bias=epsc[:, 0:1]),
                     r=["fin%d" % qs, "epsc"], w=["fin%d" % qs])
                P.op("dve", lambda e, f=f: e.reciprocal(out=f[:, 5:6], in_=f[:, 4:5]), r=["fin%d" % qs], w=["fin%d" % qs])
                P.op("dve", lambda e, qs=qs, f=f: e.scalar_tensor_tensor(out=yb[qs][:], in0=o1[qs][:], scalar=f[:, 5:6], in1=subln[:],
                                                                        op0=ALU.mult, op1=ALU.mult),
                     r=["o1_%d" % qs, "fin%d" % qs, "subln"], w=["yb%d" % qs])
                P.dma(y[blk * 128:(blk + 1) * 128, h, :], yb[qs][:], r=["yb%d" % qs], q=("sp" if qs == 0 else "act"))
    P.emit()
    P.close()
    return nc


import math

NT = 8
CH = 64


def s5_layout(a_re, a_im, log_dt, b_re, b_im, c_re, c_im, d_skip, g0):
    G = 16
    prm = np.zeros((128, 3, NT), np.float32)
    BT = np.zeros((32, 2, NT, 128), np.float32)
    CT = np.zeros((128, 2, NT, 32), np.float32)
    dsk = np.zeros((32, NT), np.float32)
    for i in range(NT):
        for gl in range(2):
            g = g0 + 2 * i + gl
            prm[gl * 64:(gl + 1) * 64, 0, i] = a_re[g]
            prm[gl * 64:(gl + 1) * 64, 1, i] = a_im[g]
            prm[gl * 64:(gl + 1) * 64, 2, i] = log_dt[g]
            BT[gl * 16:(gl + 1) * 16, 0, i, gl * 64:(gl + 1) * 64] = b_re[g].T
            BT[gl * 16:(gl + 1) * 16, 1, i, gl * 64:(gl + 1) * 64] = b_im[g].T
            CT[gl * 64:(gl + 1) * 64, 0, i, gl * 16:(gl + 1) * 16] = c_re[g].T
            CT[gl * 64:(gl + 1) * 64, 1, i, gl * 16:(gl + 1) * 16] = c_im[g].T
            dsk[gl * 16:(gl + 1) * 16, i] = d_skip[g * 16:(g + 1) * 16]
    bidx = np.ascontiguousarray(np.broadcast_to(np.arange(CH + 1, dtype=np.float32)[None], (128, CH + 1)))
    return {"prm": prm, "BT": BT, "CT": CT, "dsk": dsk, "bidx": bidx}


def build_ks5(S=4096):
    nc = bass.Bass("TRN2", target_bir_lowering=False)
    P = Prog(nc)
    TT = 512
    NTT = S // TT
    NCK = TT // CH
    uT = P.dram("uT", [32 * NT, S], F32, "ExternalInput")
    prm_d = P.dram("prm", [128, 3, NT], F32, "ExternalInput")
    BT_d = P.dram("BT", [32, 2, NT, 128], F32, "ExternalInput")
    CT_d = P.dram("CT", [128, 2, NT, 32], F32, "ExternalInput")
    dsk_d = P.dram("dsk", [32, NT], F32, "ExternalInput")
    bidx_d = P.dram("bidx", [128, CH + 1], F32, "ExternalInput")
    yT = P.dram("yT", [32 * NT, S], F32, "ExternalOutput")

    prm = P.sb("prm_sb", [128, 3, NT], F32)
    BT = P.sb("BT_sb", [32, 2, NT, 128], F32)
    CT = P.sb("CT_sb", [128, 2, NT, 32], F32)
    dsk = P.sb("dsk_sb", [32, NT], F32)
    bidx = P.sb("bidx_sb", [128, CH + 1], F32)
    u_bufs = [P.sb("u_sb%d" % i, [32, NT, 512], F32) for i in range(2)]
    sc = P.sb("sc", [128, 24, NT], F32)
    ang = P.sb("ang", [128, NT, CH + 2], F32)
    tmpi = P.sb("tmpi", [128, NT, CH + 2], I32)
    tmpf = P.sb("tmpf", [128, NT, CH + 2], F32)
    cosb = P.sb("cosb", [128, NT, CH + 2], F32)
    sinb = P.sb("sinb", [128, NT, CH + 2], F32)
    nsinb = P.sb("nsinb", [128, NT, CH + 2], F32)
    tin_re = P.sb("tin_re", [128, NT, CH], F32)
    tin_im = P.sb("tin_im", [128, NT, CH], F32)
    ttmp = P.sb("ttmp", [128, NT, CH], F32)
    rtab = P.sb("rtab", [128, NT, CH], F32)
    in_re = P.sb("in_re", [128, TT], F32)
    in_im = P.sb("in_im", [128, TT], F32)
    t1 = P.sb("t1", [128, TT], F32)
    t2 = P.sb("t2", [128, TT], F32)
    z_re = P.sb("z_re", [128, NT, TT], F32)
    z_im = P.sb("z_im", [128, NT, TT], F32)
    x_re = P.sb("x_re", [128, TT], F32)
    x_im = P.sb("x_im", [128, TT], F32)
    t3 = P.sb("t3", [128, TT], F32)
    t4 = P.sb("t4", [128, TT], F32)
    init = P.sb("init", [128, 2, NT], F32)
    it = P.sb("it", [128, 4, NT], F32)
    ysb = [P.sb("ysb%d" % i, [32, TT], F32) for i in range(2)]
    p_re = P.ps("p_re", [128, TT])
    p_im = P.ps("p_im", [128, TT])
    p_y = [P.ps("p_y%d" % i, [32, TT]) for i in range(2)]

    P.dma(prm[:], prm_d, w=["prm"])
    P.dma(BT[:], BT_d, w=["BT"])
    P.dma(CT[:], CT_d, w=["CT"])
    P.dma(dsk[:], dsk_d, w=["dsk"])
    P.dma(bidx[:], bidx_d, w=["bidx"])
    a_re, a_im = prm[:, 0, :], prm[:, 1, :]
    dt, r_, th = sc[:, 0, :], sc[:, 1, :], sc[:, 2, :]
    K = ["sc"]
    _act(P, dt, prm[:, 2, :], AF.Exp, r=["prm"], w=K)
    _tt(P, th, a_im, dt, ALU.mult, r=["prm"] + K, w=K)
    _tt(P, r_, a_re, dt, ALU.mult, r=["prm"] + K, w=K)
    _act(P, r_, r_, AF.Exp, r=K, w=K)
    for i in range(NT):
        _ts(P, ang[:, i, 0:CH + 1], bidx[:], sc[:, 2, i:i + 1], None, ALU.mult, None, r=["bidx"] + K, w=["ang"])
    _cp(P, ang[:, :, CH + 1], th, r=K, w=["ang"])
    af = lambda t: t[:].rearrange("p a b -> p (a b)")
    sincos_tables(P, af(ang), af(cosb), af(sinb), af(tmpi), af(tmpf), ["ang"], "cosb", "sinb")
    _ts(P, af(nsinb), af(sinb), -1.0, None, ALU.mult, None, r=["sinb"], w=["nsinb"])
    cth, sth = cosb[:, :, CH + 1], sinb[:, :, CH + 1]
    nr, ni, den, cr, ci, tA, tB = (sc[:, j, :] for j in range(3, 10))
    _tt(P, nr, r_, cth, ALU.mult, r=K + ["cosb"], w=K)
    _ts(P, nr, nr, -1.0, None, ALU.add, None, r=K, w=K)
    _tt(P, ni, r_, sth, ALU.mult, r=K + ["sinb"], w=K)
    _tt(P, den, a_re, a_re, ALU.mult, r=["prm"], w=K)
    _tt(P, tA, a_im, a_im, ALU.mult, r=["prm"], w=K)
    _tt(P, den, den, tA, ALU.add, r=K, w=K)
    _rcp(P, den, den, r=K, w=K)
    _tt(P, cr, nr, a_re, ALU.mult, r=K + ["prm"], w=K)
    _tt(P, tA, ni, a_im, ALU.mult, r=K + ["prm"], w=K)
    _tt(P, cr, cr, tA, ALU.add, r=K, w=K)
    _tt(P, cr, cr, den, ALU.mult, r=K, w=K)
    _tt(P, ci, ni, a_re, ALU.mult, r=K + ["prm"], w=K)
    _tt(P, tA, nr, a_im, ALU.mult, r=K + ["prm"], w=K)
    _tt(P, ci, ci, tA, ALU.subtract, r=K, w=K)
    _tt(P, ci, ci, den, ALU.mult, r=K, w=K)
    for i in range(NT):
        _ts(P, tin_re[:, i, :], cosb[:, i, 0:CH], sc[:, 6, i:i + 1], None, ALU.mult, None, r=K + ["cosb"], w=["tin_re"])
        _stt(P, tin_re[:, i, :], sinb[:, i, 0:CH], sc[:, 7, i:i + 1], tin_re[:, i, :], ALU.mult, ALU.add, r=K + ["sinb", "tin_re"], w=["tin_re"])
        _ts(P, tin_im[:, i, :], cosb[:, i, 0:CH], sc[:, 7, i:i + 1], None, ALU.mult, None, r=K + ["cosb"], w=["tin_im"])
        _ts(P, ttmp[:, i, :], sinb[:, i, 0:CH], sc[:, 6, i:i + 1], None, ALU.mult, None, r=K + ["sinb"], w=["ttmp"])
        _tt(P, tin_im[:, i, :], tin_im[:, i, :], ttmp[:, i, :], ALU.subtract, r=["tin_im", "ttmp"], w=["tin_im"])
        P.op("pool", lambda e, i=i: e.memset(rtab[:, i, :], 1.0), w=["rtab"])
        _ts(P, rtab[:, i, :], rtab[:, i, :], sc[:, 1, i:i + 1], None, ALU.mult, None, r=K + ["rtab"], w=["rtab"])
    c64, s64 = cosb[:, :, CH], sinb[:, :, CH]
    P.op("pool", lambda e: e.memset(init[:], 0.0), w=["init%d" % i for i in range(NT)])

    def bc(t, i):
        return t[:, i:i + 1, :].to_broadcast([128, NCK, CH])

    def v3(t):
        return t[:].rearrange("p (a b) -> p a b", b=CH)

    yi = 0
    for tt in range(NTT):
        tok = slice(tt * TT, (tt + 1) * TT)
        u_sb = u_bufs[tt % 2]
        uk = "u%d" % (tt % 2)
        P.dma(u_sb[:], uT.rearrange("(i c) s -> c i s", c=32)[:, :, tok], w=[uk], q="act")
        for i in range(NT):
            _mm(P, p_re[:], BT[:, 0, i, :], u_sb[:, i, :], r=["BT", uk], w=["p_re"])
            _mm(P, p_im[:], BT[:, 1, i, :], u_sb[:, i, :], r=["BT", uk], w=["p_im"])
            pr3 = p_re[:].rearrange("p (a b) -> p a b", b=CH)
            pi3 = p_im[:].rearrange("p (a b) -> p a b", b=CH)
            _tt(P, v3(in_re), pr3, bc(tin_re, i), ALU.mult, r=["p_re", "tin_re"], w=["in_re"])
            _tt(P, v3(t1), pi3, bc(tin_im, i), ALU.mult, r=["p_im", "tin_im"], w=["t1"])
            _tt(P, v3(in_im), pr3, bc(tin_im, i), ALU.mult, r=["p_re", "tin_im"], w=["in_im"])
            _tt(P, v3(t2), pi3, bc(tin_re, i), ALU.mult, r=["p_im", "tin_re"], w=["t2"])
            _tt(P, in_re[:], in_re[:], t1[:], ALU.subtract, r=["in_re", "t1"], w=["in_re"], eng="pool")
            _tt(P, in_im[:], in_im[:], t2[:], ALU.add, r=["in_im", "t2"], w=["in_im"], eng="pool")
            for a in range(NCK):
                cs = slice(a * CH, (a + 1) * CH)
                P.op("dve", lambda e, i=i, cs=cs: e.tensor_tensor_scan(out=z_re[:, i, cs], data0=rtab[:, i, :], data1=in_re[:, cs],
                                                                       initial=init[:, 0, i:i + 1], op0=ALU.mult, op1=ALU.add),
                     r=["rtab", "in_re", "init%d" % i], w=["z_re%d" % i])
                P.op("dve", lambda e, i=i, cs=cs: e.tensor_tensor_scan(out=z_im[:, i, cs], data0=rtab[:, i, :], data1=in_im[:, cs],
                                                                       initial=init[:, 1, i:i + 1], op0=ALU.mult, op1=ALU.add),
                     r=["rtab", "in_im", "init%d" % i], w=["z_im%d" % i])
                last = a * CH + CH - 1
                zr, zi = z_re[:, i, last:last + 1], z_im[:, i, last:last + 1]
                kk = ["z_re%d" % i, "z_im%d" % i, "cosb", "sinb"]
                _ts(P, it[:, 0, i:i + 1], zr, c64[:, i:i + 1], None, ALU.mult, None, r=kk, w=["it%d" % i])
                _ts(P, it[:, 1, i:i + 1], zr, s64[:, i:i + 1], None, ALU.mult, None, r=kk, w=["it%d" % i])
                _stt(P, init[:, 0, i:i + 1], zi, nsinb[:, i, CH:CH + 1], it[:, 0, i:i + 1], ALU.mult, ALU.add, r=kk + ["nsinb", "it%d" % i], w=["init%d" % i])
                _stt(P, init[:, 1, i:i + 1], zi, c64[:, i:i + 1], it[:, 1, i:i + 1], ALU.mult, ALU.add, r=kk + ["it%d" % i], w=["init%d" % i])
            zr3 = z_re[:, i, :].rearrange("p (a b) -> p a b", b=CH)
            zi3 = z_im[:, i, :].rearrange("p (a b) -> p a b", b=CH)
            cb = cosb[:, i:i + 1, 0:CH].to_broadcast([128, NCK, CH])
            sb_ = sinb[:, i:i + 1, 0:CH].to_broadcast([128, NCK, CH])
            nsb = nsinb[:, i:i + 1, 0:CH].to_broadcast([128, NCK, CH])
            _tt(P, v3(x_re), zr3, cb, ALU.mult, r=["z_re%d" % i, "cosb"], w=["x_re"], eng="pool")
            _tt(P, v3(t3), zi3, sb_, ALU.mult, r=["z_im%d" % i, "sinb"], w=["t3"], eng="pool")
            _tt(P, x_re[:], x_re[:], t3[:], ALU.subtract, r=["x_re", "t3"], w=["x_re"], eng="pool")
            _tt(P, v3(x_im), zr3, nsb, ALU.mult, r=["z_re%d" % i, "nsinb"], w=["x_im"], eng="pool")
            _tt(P, v3(t4), zi3, cb, ALU.mult, r=["z_im%d" % i, "cosb"], w=["t4"], eng="pool")
            _tt(P, x_im[:], x_im[:], t4[:], ALU.subtract, r=["x_im", "t4"], w=["x_im"], eng="pool")
            k = yi % 2
            yi += 1
            _mm(P, p_y[k][:], CT[:, 0, i, :], x_re[:], r=["CT", "x_re"], w=["p_y%d" % k], start=True, stop=False)
            _mm(P, p_y[k][:], CT[:, 1, i, :], x_im[:], r=["CT", "x_im"], w=["p_y%d" % k], start=False, stop=True)
            _stt(P, ysb[k][:], u_sb[:, i, :], dsk[:, i:i + 1], p_y[k][:], ALU.mult, ALU.add, r=[uk, "dsk", "p_y%d" % k], w=["ysb%d" % k])
            P.dma(yT[32 * i:32 * (i + 1), tok], ysb[k][:], r=["ysb%d" % k], q="sp")
    P.emit()
    P.close()
    return nc


C = 64


def gdn_consts():
    r = np.arange(C)[:, None]
    c = np.arange(C)[None, :]
    upi = (r <= c).astype(np.float32)
    los = (r > c).astype(np.float32)
    loi = (r >= c).astype(np.float32)
    return np.ascontiguousarray(np.stack([upi, los, loi], axis=1))


def build_kgdn(S=4096, NH=3):
    nc = bass.Bass("TRN2", target_bir_lowering=False)
    P = Prog(nc)
    NCH = S // C
    NB = 8
    pT = P.dram("pT", [3, NH, 128, S], F32, "ExternalInput")
    cw_d = P.dram("cw", [128, 3, NH, 4], F32, "ExternalInput")
    zt_d = P.dram("zt", [C, NH, NCH, 128], F32, "ExternalInput")
    ab_d = P.dram("ab", [C, 2, NH, NCH], F32, "ExternalInput")
    hp_d = P.dram("hp", [C, 2, NH], F32, "ExternalInput")
    gnw_d = P.dram("gnw", [C, 128], F32, "ExternalInput")
    msk_d = P.dram("msk", [C, 3, C], F32, "ExternalInput")
    id_d = P.dram("ident", [128, 128], F32, "ExternalInput")
    y = P.dram("y", [S, NH, 128], F32, "ExternalOutput")

    cw = P.sb("cw_sb", [128, 3, NH, 4], F32)
    ab = P.sb("ab_sb", [C, 2, NH, NCH], F32)
    hp = P.sb("hp_sb", [C, 2, NH], F32)
    gnw = P.sb("gnw_sb", [C, 128], F32)
    msk = P.sb("msk_sb", [C, 3, C], F32)
    ident = P.sb("id_sb", [128, 128], F32)
    ones = P.sb("ones", [C, 128], F32)
    epsc = P.sb("epsc", [128, 1], F32)
    g = P.sb("g", [C, NH, NCH], F32)
    beta = P.sb("beta", [C, NH, NCH], F32)
    egc = P.sb("egc", [C, NH, NCH], F32)
    gcs = P.sb("gcs", [C, NH * NCH], F32)
    edec = P.sb("edec", [C, NH, NCH], F32)
    egl = P.sb("egl", [128, NH, NCH], F32)
    raw = P.sb("raw", [128, S + 4], F32)
    cv = [P.sb("cv%d" % i, [128, S], F32) for i in range(3)]
    zt = P.sb("zt_sb", [C, NB, 128], F32)
    qkv = P.sb("qkv", [C, NB, 3, 128], F32)
    aux = P.sb("aux", [C, NB, 3, 128], F32)
    st = P.sb("st", [C, NB, 8], F32)
    junk = P.sb("junk", [C, 128], F32)
    knT = P.sb("knT", [128, NB, C], F32)
    qnT = P.sb("qnT", [128, NB, C], F32)
    qgT = P.sb("qgT", [128, NB, C], F32)
    wT = P.sb("wT", [128, NB, C], F32)
    G2 = P.sb("G2", [C, NB, C], F32)
    E = P.sb("E", [C, NB, C], F32)
    ET = P.sb("ET", [C, NB, C], F32)
    Lm = P.sb("Lm", [C, NB, C], F32)
    Am = P.sb("Am", [C, NB, C], F32)
    Pm = [P.sb("Pm%d" % i, [C, NB, C], F32) for i in range(2)]
    Qm = [P.sb("Qm%d" % i, [C, NB, C], F32) for i in range(2)]
    X = P.sb("X", [C, NB, C], F32)
    QKT = P.sb("QKT", [C, NB, C], F32)
    uw = P.sb("uw", [C, NB, 256], F32)
    Sst = P.sb("Sst", [128, 128], F32)
    vnew = P.sb("vnew", [C, 128], F32)
    osb = [P.sb("osb%d" % i, [C, 128], F32) for i in range(2)]
    ost = P.sb("ost", [C, 8], F32)
    zs = P.sb("zs", [C, NB, 128], F32)
    pa = P.ps("pa", [128, 512])
    pb = P.ps("pb", [128, 512])
    pc = P.ps("pc", [128, 512])
    pd = P.ps("pd", [128, 512])
    pws = P.ps("pws", [C, 128])
    po = P.ps("po", [C, 128])
    pds = P.ps("pds", [128, 128])
    pg = P.ps("pg", [128, 512])

    P.dma(cw[:], cw_d, w=["cw"])
    P.dma(ab[:], ab_d, w=["ab"])
    P.dma(hp[:], hp_d, w=["hp"])
    P.dma(gnw[:], gnw_d, w=["gnw"])
    P.dma(msk[:], msk_d, w=["msk"])
    P.dma(ident[:], id_d, w=["ident"])
    P.op("pool", lambda e: e.memset(ones[:], 1.0), w=["ones"])
    P.op("pool", lambda e: e.memset(epsc[:], 1e-6), w=["epsc"])
    UPI, LOS, LOI = msk[:, 0, :], msk[:, 1, :], msk[:, 2, :]

    _act(P, hp[:, 0, :], hp[:, 0, :], AF.Exp, r=["hp"], w=["hp"])
    for h in range(NH):
        _act(P, g[:, h, :], ab[:, 0, h, :], AF.Exp, r=["ab", "hp"], w=["g"], bias=hp[:, 1, h:h + 1])
        _act(P, g[:, h, :], g[:, h, :], AF.Ln, r=["g"], w=["g"], bias=1.0)
        _ts(P, g[:, h, :], g[:, h, :], hp[:, 0, h:h + 1], -1.0, ALU.mult, ALU.mult, r=["g", "hp"], w=["g"])
    _act(P, beta[:].rearrange("p h n -> p (h n)"), ab[:, 1, :, :].rearrange("p h n -> p (h n)"), AF.Sigmoid, r=["ab"], w=["beta"])
    NF = NH * NCH
    gf = g[:].rearrange("p h n -> p (h n)")
    _mm(P, pg[0:C, 0:NF], UPI, gf, r=["msk", "g"], w=["pg"])
    _mm(P, pg[0:C, 256:256 + NF], ones[:, 0:C], gf, r=["ones", "g"], w=["pg"])
    _mm(P, pb[:, 0:NF], ones[:, :], gf, r=["ones", "g"], w=["pb"])
    _cp(P, gcs[:], pg[0:C, 0:NF], r=["pg"], w=["gcs"])
    _act(P, egc[:].rearrange("p h n -> p (h n)"), gcs[:], AF.Exp, r=["gcs"], w=["egc"])
    _tt(P, edec[:].rearrange("p h n -> p (h n)"), pg[0:C, 256:256 + NF], gcs[:], ALU.subtract, r=["pg", "gcs"], w=["edec"])
    _act(P, edec[:].rearrange("p h n -> p (h n)"), edec[:].rearrange("p h n -> p (h n)"), AF.Exp, r=["edec"], w=["edec"])
    _act(P, egl[:].rearrange("p h n -> p (h n)"), pb[:, 0:NF], AF.Exp, r=["pb"], w=["egl"])

    for h in range(NH):
        for t in range(3):
            P.op("pool", lambda e: e.memset(raw[:, 0:4], 0.0), w=["raw"])
            P.dma(raw[:, 4:4 + S], pT[t, h], w=["raw"])
            _ts(P, cv[t][:], raw[:, 4:4 + S], cw[:, t, h, 3:4], None, ALU.mult, None, r=["raw", "cw"], w=["cv%d" % t])
            for j in range(3):
                _stt(P, cv[t][:], raw[:, 1 + j:1 + j + S], cw[:, t, h, j:j + 1], cv[t][:], ALU.mult, ALU.add,
                     r=["raw", "cw", "cv%d" % t], w=["cv%d" % t], eng="dve")
            _act(P, cv[t][:], cv[t][:], AF.Silu, r=["cv%d" % t], w=["cv%d" % t])
        P.op("pool", lambda e: e.memset(Sst[:], 0.0), w=["S"])
        for b0 in range(0, NCH, NB):
            P.dma(zt[:], zt_d[:, h, b0:b0 + NB, :], w=["zt"], q="act")
            for i in range(NB):
                n = b0 + i
                ps_t = (pa, pb)[i % 2]
                kt = "pa" if i % 2 == 0 else "pb"
                for t in range(3):
                    _tr(P, ps_t[0:C, t * 128:(t + 1) * 128], cv[t][:, n * C:(n + 1) * C], ident[:], r=["cv%d" % t, "ident"], w=[kt])
                for t in range(2):
                    _act(P, junk[:], ps_t[0:C, t * 128:(t + 1) * 128], AF.Square, r=[kt], w=["junk", "st"], accum_out=st[:, i, t:t + 1])
                _act(P, st[:, i, 2:4], st[:, i, 0:2], AF.Sqrt, r=["st", "epsc"], w=["st"], bias=epsc[0:C, 0:1])
                _rcp(P, st[:, i, 4:6], st[:, i, 2:4], r=["st"], w=["st"])
                _ts(P, qkv[:, i, 0, :], ps_t[0:C, 0:128], st[:, i, 4:5], float(128 ** -0.5), ALU.mult, ALU.mult, r=[kt, "st"], w=["qkv"])
                _ts(P, qkv[:, i, 1, :], ps_t[0:C, 128:256], st[:, i, 5:6], None, ALU.mult, None, r=[kt, "st"], w=["qkv"])
                _ts(P, qkv[:, i, 2, :], ps_t[0:C, 256:384], beta[:, h, n:n + 1], None, ALU.mult, None, r=[kt, "beta"], w=["qkv"])
                _ts(P, aux[:, i, 0, :], qkv[:, i, 1, :], beta[:, h, n:n + 1], egc[:, h, n:n + 1], ALU.mult, ALU.mult, r=["qkv", "beta", "egc"], w=["aux"], eng="pool")
                _ts(P, aux[:, i, 1, :], qkv[:, i, 0, :], egc[:, h, n:n + 1], None, ALU.mult, None, r=["qkv", "egc"], w=["aux"], eng="pool")
                _ts(P, aux[:, i, 2, :], qkv[:, i, 1, :], edec[:, h, n:n + 1], None, ALU.mult, None, r=["qkv", "edec"], w=["aux"], eng="pool")
                _ts(P, G2[:, i, :], LOS, g[:, h, n:n + 1], None, ALU.mult, None, r=["msk", "g"], w=["G2"], eng="pool")
            for (src_t, src_j, dst, dk, pst, pk) in ((qkv, 1, knT, "knT", pc, "pc"), (qkv, 0, qnT, "qnT", pd, "pd"), (aux, 1, qgT, "qgT", pc, "pc")):
                srck = "qkv" if src_t is qkv else "aux"
                for i in range(NB):
                    _tr(P, pst[:, i * C:(i + 1) * C], src_t[:, i, src_j, :], ident[0:C, 0:C], r=[srck, "ident"], w=[pk])
                _cp(P, dst[:].rearrange("p a b -> p (a b)"), pst[:, 0:NB * C], r=[pk], w=[dk], eng=("act" if dk == "qnT" else "dve"))
            for i in range(NB):
                _mm(P, pa[0:C, i * C:(i + 1) * C], UPI, G2[:, i, :], r=["msk", "G2"], w=["pa"])
            for i in range(NB):
                _mm(P, pb[0:C, i * C:(i + 1) * C], G2[:, i, :], UPI, r=["msk", "G2"], w=["pb"])
            _act(P, E[:].rearrange("p a b -> p (a b)"), pa[0:C, 0:NB * C], AF.Exp, r=["pa"], w=["E"])
            _act(P, ET[:].rearrange("p a b -> p (a b)"), pb[0:C, 0:NB * C], AF.Exp, r=["pb"], w=["ET"])
            for i in range(NB):
                _mm(P, pc[0:C, i * C:(i + 1) * C], knT[:, i, :], knT[:, i, :], r=["knT"], w=["pc"])
            for i in range(NB):
                _mm(P, pd[0:C, i * C:(i + 1) * C], knT[:, i, :], qnT[:, i, :], r=["knT", "qnT"], w=["pd"])
            for i in range(NB):
                n = b0 + i
                _stt(P, Lm[:, i, :], pc[0:C, i * C:(i + 1) * C], beta[:, h, n:n + 1], E[:, i, :], ALU.mult, ALU.mult, r=["pc", "beta", "E"], w=["Lm"])
                _tt(P, Lm[:, i, :], Lm[:, i, :], LOS, ALU.mult, r=["Lm", "msk"], w=["Lm"], eng="pool")
                _tt(P, QKT[:, i, :], pd[0:C, i * C:(i + 1) * C], ET[:, i, :], ALU.mult, r=["pd", "ET"], w=["QKT"])
                _tt(P, QKT[:, i, :], QKT[:, i, :], UPI, ALU.mult, r=["QKT", "msk"], w=["QKT"], eng="pool")
            for i in range(NB):
                _tr(P, pa[0:C, i * C:(i + 1) * C], Lm[:, i, :], ident[0:C, 0:C], r=["Lm", "ident"], w=["pa"])
            _cp(P, Am[:].rearrange("p a b -> p (a b)"), pa[0:C, 0:NB * C], r=["pa"], w=["Am"])
            for i in range(NB):
                _tt(P, X[:, i, :], ident[0:C, 0:C], Am[:, i, :], ALU.subtract, r=["ident", "Am"], w=["X"], eng="pool")
            Pc, Qc, pk_, qk_ = Am, Lm, "Am", "Lm"
            for lvl in range(5):
                Pn, Qn = Pm[lvl % 2], Qm[lvl % 2]
                pnk, qnk = "Pm%d" % (lvl % 2), "Qm%d" % (lvl % 2)
                for i in range(NB):
                    _mm(P, pa[0:C, i * C:(i + 1) * C], Qc[:, i, :], Pc[:, i, :], r=[pk_, qk_], w=["pa"])
                for i in range(NB):
                    _mm(P, pb[0:C, i * C:(i + 1) * C], Pc[:, i, :], Qc[:, i, :], r=[pk_, qk_], w=["pb"])
                _cp(P, Pn[:].rearrange("p a b -> p (a b)"), pa[0:C, 0:NB * C], r=["pa"], w=[pnk])
                _cp(P, Qn[:].rearrange("p a b -> p (a b)"), pb[0:C, 0:NB * C], r=["pb"], w=[qnk], eng="act")
                for i in range(NB):
                    _mm(P, pc[0:C, i * C:(i + 1) * C], Qn[:, i, :], X[:, i, :], r=[qnk, "X"], w=["pc"])
                _tt(P, X[:].rearrange("p a b -> p (a b)"), X[:].rearrange("p a b -> p (a b)"), pc[0:C, 0:NB * C], ALU.add, r=["X", "pc"], w=["X"])
                Pc, Qc, pk_, qk_ = Pn, Qn, pnk, qnk
            for i in range(NB):
                pst, pk = ((pa, "pa"), (pb, "pb"))[(i // 2) % 2]
                _mm(P, pst[0:C, (i % 2) * 256:(i % 2) * 256 + 128], X[:, i, :], qkv[:, i, 2, :], r=["X", "qkv"], w=[pk])
                _mm(P, pst[0:C, (i % 2) * 256 + 128:(i % 2) * 256 + 256], X[:, i, :], aux[:, i, 0, :], r=["X", "aux"], w=[pk])
                if i % 2 == 1:
                    _cp(P, uw[:, i - 1:i + 1, :].rearrange("p a b -> p (a b)"), pst[0:C, 0:512], r=[pk], w=["uw"], eng=("dve" if (i // 2) % 2 == 0 else "act"))
            for i in range(NB):
                _tr(P, pc[:, i * C:(i + 1) * C], uw[:, i, 128:256], ident[0:C, 0:C], r=["uw", "ident"], w=["pc"])
            _cp(P, wT[:].rearrange("p a b -> p (a b)"), pc[:, 0:NB * C], r=["pc"], w=["wT"])
            _act(P, zs[:].rearrange("p a b -> p (a b)"), zt[:].rearrange("p a b -> p (a b)"), AF.Silu, r=["zt"], w=["zs"])
            for i in range(NB):
                n = b0 + i
                ob = osb[i % 2]
                obk = "osb%d" % (i % 2)
                _mm(P, pws[:], wT[:, i, :], Sst[:], r=["wT", "S"], w=["pws"])
                _tt(P, vnew[:], uw[:, i, 0:128], pws[:], ALU.subtract, r=["uw", "pws"], w=["vnew"])
                _mm(P, po[:], qgT[:, i, :], Sst[:], r=["qgT", "S"], w=["po"], start=True, stop=False)
                _mm(P, po[:], QKT[:, i, :], vnew[:], r=["QKT", "vnew"], w=["po"], start=False, stop=True)
                _mm(P, pds[:], aux[:, i, 2, :], vnew[:], r=["aux", "vnew"], w=["pds"])
                _stt(P, Sst[:], Sst[:], egl[:, h, n:n + 1], pds[:], ALU.mult, ALU.add, r=["S", "egl", "pds"], w=["S"])
                _act(P, junk[:], po[:], AF.Square, r=["po"], w=["junk", "ost"], accum_out=ost[:, 0:1])
                _act(P, ost[:, 1:2], ost[:, 0:1], AF.Sqrt, r=["ost", "epsc"], w=["ost"], scale=1.0 / 128.0, bias=epsc[0:C, 0:1])
                _rcp(P, ost[:, 2:3], ost[:, 1:2], r=["ost"], w=["ost"])
                _stt(P, ob[:], po[:], ost[:, 2:3], gnw[:], ALU.mult, ALU.mult, r=["po", "ost", "gnw"], w=[obk])
                _tt(P, ob[:], ob[:], zs[:, i, :], ALU.mult, r=[obk, "zs"], w=[obk], eng="pool")
                P.dma(y[n * C:(n + 1) * C, h, :], ob[:], r=[obk], q="sp")
    P.emit()
    P.close()
    return nc


D = 2048
KC = 16


def build_kb(NTOK=2048, NPASS=1024, DFF=5632, NE=1, moe=False, final=False):
    nc = bass.Bass("TRN2", target_bir_lowering=False)
    P = Prog(nc)
    NP = NPASS
    NH = NP // 512
    xT = P.dram("xT", [D, NTOK], F32, "ExternalInput")
    yT = P.dram("yT", [D, NTOK], F32, "ExternalInput")
    wglu = P.dram("wglu", [512, 512], F32, "ExternalInput")
    wout = P.dram("wout", [D, D], F32, "ExternalInput")
    vecs_d = P.dram("vecs", [128, 6, KC], F32, "ExternalInput")
    w1 = P.dram("w1", [NE, D, DFF], F32, "ExternalInput")
    w3 = P.dram("w3", [NE, D, DFF], F32, "ExternalInput")
    w2 = P.dram("w2", [NE, DFF, D], F32, "ExternalInput")
    if moe:
        wr_d = P.dram("wr", [128, KC, 8], F32, "ExternalInput")
        sel_d = P.dram("sel", [8, 8, 128], F32, "ExternalInput")
        id_d = P.dram("ident", [128, 128], F32, "ExternalInput")
    out = P.dram("out", [D, NTOK], F32, "ExternalOutput")

    vecs = P.sb("vecs_sb", [128, 6, KC], F32)
    A2 = P.sb("A2", [128, KC], F32)
    ones = P.sb("ones", [128, 128], F32)
    epsc = P.sb("epsc", [128, 1], F32)
    x1T = P.sb("x1T", [128, KC, NP], F32)
    bufA = P.sb("bufA", [128, KC, NP], BF16)
    ysf = P.sb("ysf", [128, 4, 512], F32)
    ysg = P.sb("ysg", [128, 4, 512], F32)
    wg_sb = P.sb("wg_sb", [128, 4, 512], BF16)
    WA = [P.sb("WA%d" % i, [128, KC, 256], BF16) for i in range(2)]
    WB = [P.sb("WB%d" % i, [128, KC, 256], BF16) for i in range(2)]
    WC = [P.sb("WC%d" % i, [128, 2, D], BF16) for i in range(2)]
    s_sb = [P.sb("s_sb%d" % i, [128, 512], BF16) for i in range(2)]
    a_sb = [P.sb("a_sb%d" % i, [128, 2, 512], BF16) for i in range(2)]
    sq = [P.sb("sq%d" % i, [128, 512], F32) for i in range(2)]
    rstd = P.sb("rstd", [128, 512], F32)
    ph1 = [P.ps("ph1_%d" % i, [128, 512]) for i in range(2)]
    ph3 = [P.ps("ph3_%d" % i, [128, 512]) for i in range(2)]
    po = [P.ps("po%d" % i, [128, 512]) for i in range(2)]
    pn = [P.ps("pn%d" % i, [128, 512]) for i in range(2)]
    if moe:
        wr = P.sb("wr_sb", [128, KC, 8], F32)
        sel = P.sb("sel_sb", [8, 8, 128], F32)
        ident = P.sb("id_sb", [128, 128], F32)
        comb = P.sb("comb", [128, 8, NP], BF16)
        lgT = P.sb("lgT", [8, 512], F32)
        lg = P.sb("lg", [128, 4, 8], F32)
        mk = P.sb("mk", [128, 4, 8], F32)
        e1 = P.sb("e1", [128, 4, 8], F32)
        e2 = P.sb("e2", [128, 4, 8], F32)
        m12 = P.sb("m12", [128, 4, 4], F32)
        cmb = P.sb("cmb", [128, 4, 8], F32)
        cT = P.sb("cT", [8, 512], F32)
        gT = comb[:, 0:4, :]
        gk = "comb"
    else:
        gT = P.sb("gT", [128, 4, NP], BF16)
        gk = "gT"

    P.dma(vecs[:], vecs_d, w=["vecs"])
    P.op("dve", lambda e: e.memset(ones[:], 1.0), w=["ones"])
    P.op("dve", lambda e: e.memset(epsc[:], 1e-6), w=["epsc"])
    _stt(P, A2[:], vecs[:, 1, :], 1.0, vecs[:, 0, :], ALU.add, ALU.mult, r=["vecs"], w=["A2"])
    P.dma(wg_sb[:], wglu.rearrange("(kc p) n -> p kc n", p=128), w=["wg"], q="pool")
    if moe:
        P.dma(wr[:], wr_d, w=["wr"])
        P.dma(sel[:], sel_d, w=["sel"])
        P.dma(ident[:], id_d, w=["ident"])
    xv = xT.rearrange("(kc p) t -> p kc t", p=128)
    yv = yT.rearrange("(kc p) t -> p kc t", p=128)
    ov = out.rearrange("(kc p) t -> p kc t", p=128)
    woutv = wout.rearrange("(kc p) n -> p kc n", p=128)
    NG = DFF // 256
    cnt = dict(w=0, h=0, o=0, n=0, s=0, a=0)

    def rms_stats(src_key):
        pass

    for ps_ in range(NTOK // NP):
        t0 = ps_ * NP
        for q in range(4):
            P.dma(x1T[:, q * 4:(q + 1) * 4, :], xv[:, q * 4:(q + 1) * 4, t0:t0 + NP], w=["x1T"], q=("sp" if q % 2 == 0 else "act"))
        P.dma(bufA[:, 4:KC, :], yv[:, 4:KC, t0:t0 + NP], w=["bufA"], q="pool")
        for hf in range(NH):
            tk = slice(hf * 512, (hf + 1) * 512)
            P.dma(ysf[:], yv[:, 0:4, t0 + hf * 512:t0 + (hf + 1) * 512], w=["ysf"])
            _tt(P, ysg[:], ysf[:], ysf[:], ALU.mult, r=["ysf"], w=["ysg"])
            _ts(P, ysg[:], ysg[:], 0.044715, 1.0, ALU.mult, ALU.add, r=["ysg"], w=["ysg"])
            _tt(P, ysg[:], ysg[:], ysf[:], ALU.mult, r=["ysg", "ysf"], w=["ysg"])
            _act(P, ysg[:], ysg[:], AF.Tanh, r=["ysg"], w=["ysg"], scale=float(np.sqrt(2.0 / np.pi)))
            _ts(P, ysg[:], ysg[:], 1.0, 0.5, ALU.add, ALU.mult, r=["ysg"], w=["ysg"])
            _tt(P, gT[:, :, tk], ysg[:], ysf[:], ALU.mult, r=["ysg", "ysf"], w=[gk])
            for oc in range(4):
                k = cnt["n"] % 2
                cnt["n"] += 1
                for kc in range(4):
                    _mm(P, pn[k][:], wg_sb[:, kc, oc * 128:(oc + 1) * 128], gT[:, kc, tk], r=["wg", gk], w=["pn%d" % k],
                        start=(kc == 0), stop=(kc == 3))
                s = cnt["s"] % 2
                cnt["s"] += 1
                _act(P, s_sb[s][:], pn[k][:], AF.Sigmoid, r=["pn%d" % k], w=["s_sb%d" % s])
                _tt(P, bufA[:, oc, tk], s_sb[s][:], gT[:, oc, tk], ALU.mult, r=["s_sb%d" % s, gk], w=["bufA"])
        for og in range(D // 256):
            wi = cnt["w"] % 2
            cnt["w"] += 1
            P.dma(WA[wi][:], woutv[:, :, og * 256:(og + 1) * 256], w=["WA%d" % wi], q="pool")
            for o2 in range(2):
                oc = og * 2 + o2
                for hf in range(NH):
                    tk = slice(hf * 512, (hf + 1) * 512)
                    k = cnt["o"] % 2
                    cnt["o"] += 1
                    for kc in range(KC):
                        _mm(P, po[k][:], WA[wi][:, kc, o2 * 128:(o2 + 1) * 128], bufA[:, kc, tk], r=["WA%d" % wi, "bufA"], w=["po%d" % k],
                            start=(kc == 0), stop=(kc == KC - 1))
                    _stt(P, x1T[:, oc, tk], po[k][:], vecs[:, 3, oc:oc + 1], x1T[:, oc, tk], ALU.mult, ALU.add,
                         r=["po%d" % k, "vecs", "x1T"], w=["x1T"])
        for hf in range(NH):
            tk = slice(hf * 512, (hf + 1) * 512)
            k = cnt["n"] % 2
            cnt["n"] += 1
            for kc in range(KC):
                s = kc % 2
                _act(P, sq[s][:], x1T[:, kc, tk], AF.Square, r=["x1T"], w=["sq%d" % s])
                _mm(P, pn[k][:], ones[:], sq[s][:], r=["ones", "sq%d" % s], w=["pn%d" % k], start=(kc == 0), stop=(kc == KC - 1))
            _act(P, rstd[:], pn[k][:], AF.Sqrt, r=["pn%d" % k, "epsc"], w=["rstd"], scale=1.0 / D, bias=epsc[:, 0:1])
            _rcp(P, rstd[:], rstd[:], r=["rstd"], w=["rstd"])
            k2 = cnt["n"] % 2
            cnt["n"] += 1
            for kc in range(KC):
                s = kc % 2
                _stt(P, sq[s][:], x1T[:, kc, tk], A2[:, kc:kc + 1], rstd[:], ALU.mult, ALU.mult, r=["x1T", "A2", "rstd"], w=["sq%d" % s])
                _act(P, sq[s][:], sq[s][:], AF.Identity, r=["sq%d" % s, "vecs"], w=["sq%d" % s], bias=vecs[:, 2, kc:kc + 1])
                _cp(P, bufA[:, kc, tk], sq[s][:], r=["sq%d" % s], w=["bufA"], eng="dve")
                if moe:
                    _mm(P, pn[k2][0:8, :], wr[:, kc, :], sq[s][:], r=["wr", "sq%d" % s], w=["pn%d" % k2], start=(kc == 0), stop=(kc == KC - 1))
            if moe:
                _cp(P, lgT[:], pn[k2][0:8, :], r=["pn%d" % k2], w=["lgT"])
                k3 = cnt["n"] % 2
                cnt["n"] += 1
                for j in range(4):
                    _tr(P, pn[k3][:, j * 8:(j + 1) * 8], lgT[:, j * 128:(j + 1) * 128], ident[0:8, 0:8], r=["lgT", "ident"], w=["pn%d" % k3])
                _cp(P, lg[:].rearrange("p a b -> p (a b)"), pn[k3][:, 0:32], r=["pn%d" % k3], w=["lg"])
                P.op("dve", lambda e: e.tensor_reduce(out=m12[:, :, 0], in_=lg[:], axis=AX.X, op=ALU.max), r=["lg"], w=["m12"])
                _tt(P, e1[:], lg[:], m12[:, :, 0:1].to_broadcast([128, 4, 8]), ALU.is_equal, r=["lg", "m12"], w=["e1"])
                _stt(P, mk[:], e1[:], -1e30, lg[:], ALU.mult, ALU.add, r=["e1", "lg"], w=["mk"])
                P.op("dve", lambda e: e.tensor_reduce(out=m12[:, :, 1], in_=mk[:], axis=AX.X, op=ALU.max), r=["mk"], w=["m12"])
                _tt(P, e2[:], mk[:], m12[:, :, 1:2].to_broadcast([128, 4, 8]), ALU.is_equal, r=["mk", "m12"], w=["e2"])
                _tt(P, m12[:, :, 2], m12[:, :, 1], m12[:, :, 0], ALU.subtract, r=["m12"], w=["m12"])
                _act(P, m12[:, :, 2], m12[:, :, 2], AF.Exp, r=["m12"], w=["m12"])
                _ts(P, m12[:, :, 2], m12[:, :, 2], 1.0, None, ALU.add, None, r=["m12"], w=["m12"])
                _rcp(P, m12[:, :, 2], m12[:, :, 2], r=["m12"], w=["m12"])
                _ts(P, m12[:, :, 3], m12[:, :, 2], -1.0, 1.0, ALU.mult, ALU.add, r=["m12"], w=["m12"])
                _tt(P, cmb[:], e1[:], m12[:, :, 2:3].to_broadcast([128, 4, 8]), ALU.mult, r=["e1", "m12"], w=["cmb"])
                _tt(P, e2[:], e2[:], m12[:, :, 3:4].to_broadcast([128, 4, 8]), ALU.mult, r=["e2", "m12"], w=["e2"])
                _tt(P, cmb[:], cmb[:], e2[:], ALU.add, r=["cmb", "e2"], w=["cmb"])
                k4 = cnt["n"] % 2
                cnt["n"] += 1
                for j in range(4):
                    _tr(P, pn[k4][0:8, j * 128:(j + 1) * 128], cmb[:, j, :], ident[:], r=["cmb", "ident"], w=["pn%d" % k4])
                _cp(P, cT[:], pn[k4][0:8, :], r=["pn%d" % k4], w=["cT"])
                for ex in range(8):
                    k5 = cnt["n"] % 2
                    cnt["n"] += 1
                    _mm(P, pn[k5][:], sel[:, ex, :], cT[:], r=["sel", "cT"], w=["pn%d" % k5])
                    _cp(P, comb[:, ex, tk], pn[k5][:], r=["pn%d" % k5], w=["comb"], eng=("act" if ex % 2 == 0 else "dve"))
        jobs = [(ex, g) for ex in range(NE) for g in range(NG)]

        def load_w(job, wi):
            ex, g = job
            P.dma(WA[wi][:], w1[ex].rearrange("(kc p) n -> p kc n", p=128)[:, :, g * 256:(g + 1) * 256], w=["WA%d" % wi], q="pool")
            P.dma(WB[wi][:], w3[ex].rearrange("(kc p) n -> p kc n", p=128)[:, :, g * 256:(g + 1) * 256], w=["WB%d" % wi], q="pool")
            P.dma(WC[wi][:], w2[ex, g * 256:(g + 1) * 256, :].rearrange("(c p) n -> p c n", p=128), w=["WC%d" % wi], q="pool")

        wi0 = cnt["w"] % 2
        load_w(jobs[0], wi0)
        for ji, job in enumerate(jobs):
            ex, g = job
            wi = (wi0 + ji) % 2
            if ji + 1 < len(jobs):
                load_w(jobs[ji + 1], (wi + 1) % 2)
            for hf in range(NH):
                tk = slice(hf * 512, (hf + 1) * 512)
                ai = cnt["a"] % 2
                cnt["a"] += 1
                for c in range(2):
                    hk = cnt["h"] % 2
                    cnt["h"] += 1
                    for kc in range(KC):
                        _mm(P, ph1[hk][:], WA[wi][:, kc, c * 128:(c + 1) * 128], bufA[:, kc, tk], r=["WA%d" % wi, "bufA"], w=["ph1_%d" % hk],
                            start=(kc == 0), stop=(kc == KC - 1))
                    for kc in range(KC):
                        _mm(P, ph3[hk][:], WB[wi][:, kc, c * 128:(c + 1) * 128], bufA[:, kc, tk], r=["WB%d" % wi, "bufA"], w=["ph3_%d" % hk],
                            start=(kc == 0), stop=(kc == KC - 1))
                    s = cnt["s"] % 2
                    cnt["s"] += 1
                    _act(P, s_sb[s][:], ph1[hk][:], AF.Silu, r=["ph1_%d" % hk], w=["s_sb%d" % s])
                    _tt(P, a_sb[ai][:, c, :], s_sb[s][:], ph3[hk][:], ALU.mult, r=["s_sb%d" % s, "ph3_%d" % hk], w=["a_sb%d" % ai])
                    if moe:
                        _tt(P, a_sb[ai][:, c, :], a_sb[ai][:, c, :], comb[:, ex, tk], ALU.mult, r=["a_sb%d" % ai, "comb"], w=["a_sb%d" % ai])
                for oc in range(KC):
                    k = cnt["o"] % 2
                    cnt["o"] += 1
                    for c in range(2):
                        _mm(P, po[k][:], WC[wi][:, c, oc * 128:(oc + 1) * 128], a_sb[ai][:, c, :], r=["WC%d" % wi, "a_sb%d" % ai], w=["po%d" % k],
                            start=(c == 0), stop=(c == 1))
                    _stt(P, x1T[:, oc, tk], po[k][:], vecs[:, 4, oc:oc + 1], x1T[:, oc, tk], ALU.mult, ALU.add,
                         r=["po%d" % k, "vecs", "x1T", "x1T%d" % oc], w=["x1T%d" % oc])
        cnt["w"] += len(jobs)
        fin_keys = ["x1T"] + ["x1T%d" % oc for oc in range(KC)]
        P.op("dve", lambda e: e.memset(epsc[:], 1e-6), r=fin_keys, w=["epsc", "x1T"])
        fin_keys = ["x1T"]
        if final:
            for hf in range(NH):
                tk = slice(hf * 512, (hf + 1) * 512)
                k = cnt["n"] % 2
                cnt["n"] += 1
                for kc in range(KC):
                    s = kc % 2
                    _act(P, sq[s][:], x1T[:, kc, tk], AF.Square, r=fin_keys, w=["sq%d" % s])
                    _mm(P, pn[k][:], ones[:], sq[s][:], r=["ones", "sq%d" % s], w=["pn%d" % k], start=(kc == 0), stop=(kc == KC - 1))
                _act(P, rstd[:], pn[k][:], AF.Sqrt, r=["pn%d" % k, "epsc"], w=["rstd"], scale=1.0 / D, bias=epsc[:, 0:1])
                _rcp(P, rstd[:], rstd[:], r=["rstd"], w=["rstd"])
                for kc in range(KC):
                    _stt(P, x1T[:, kc, tk], x1T[:, kc, tk], vecs[:, 5, kc:kc + 1], rstd[:], ALU.mult, ALU.mult, r=fin_keys + ["vecs", "rstd"], w=fin_keys)
        for q in range(4):
            P.dma(ov[:, q * 4:(q + 1) * 4, t0:t0 + NP], x1T[:, q * 4:(q + 1) * 4, :], r=["x1T"], q=("sp" if q % 2 == 0 else "act"))
    P.emit()
    P.close()
    return nc


NCORES = 8
_CORES = list(range(NCORES))


def _run(nc, in_maps):
    res = run_bass_kernel_spmd(nc, in_maps, core_ids=_CORES)
    return res.results


def kernel(x, c, positions, w_ada, b_ada, norm_mix, norm_ffn, norm_final, w_in, w_out,
           ssm_a_re, ssm_a_im, ssm_log_dt, ssm_b_re, ssm_b_im, ssm_c_re, ssm_c_im, ssm_d, ssm_w_glu,
           diff_lam_q1, diff_lam_k1, diff_lam_q2, diff_lam_k2, diff_subln,
           gdn_conv, gdn_a_log, gdn_dt_bias, gdn_norm,
           ffn_w1, ffn_w3, ffn_w2, moe_router, moe_w1, moe_w3, moe_w2):
    f32 = np.float32
    x = np.asarray(x, f32)
    B, S, Dm = x.shape
    HALF = S // 2
    mod = run_k0(np.asarray(c, f32), np.asarray(w_ada, f32), np.asarray(b_ada, f32))
    ident = np.eye(128, dtype=f32)
    invf, sgn, tri = att_consts()
    perm = rope_perm_index()
    msk = gdn_consts()
    sel = np.zeros((8, 8, 128), f32)
    for e in range(8):
        sel[e, e, :] = 1
    xcur = x
    for l in range(2):
        shift1, scale1, gate1, shift2, scale2, gate2 = [mod[l][:, j * 2048:(j + 1) * 2048] for j in range(6)]
        nc = build_ka(HALF, 5900)
        wl = np.ascontiguousarray(w_in[l], dtype=f32)
        in_maps = []
        for i in range(NCORES):
            b, hh = divmod(i, 2)
            vecs = np.ascontiguousarray(np.stack([pscal(norm_mix[l]), pscal(scale1[b]), pscal(shift1[b])], axis=1))
            in_maps.append({"x": np.ascontiguousarray(xcur[b, hh * HALF:(hh + 1) * HALF]), "w": wl, "vecs": vecs, "ident": ident})
        r = _run(nc, in_maps)
        proj = np.empty((B, S, 5900), f32)
        for i in range(NCORES):
            b, hh = divmod(i, 2)
            proj[b, hh * HALF:(hh + 1) * HALF] = r[i]["out"]
        del r
        u = proj[:, :, 0:512]
        dq = proj[:, :, 512:1280].reshape(B, S, 6, 128)
        dk = proj[:, :, 1280:2048].reshape(B, S, 6, 128)
        dv = proj[:, :, 2048:2816].reshape(B, S, 6, 128)
        gqkv = proj[:, :, 2816:5120]
        gz = proj[:, :, 5120:5888]
        ga = proj[:, :, 5888:5894]
        gb = proj[:, :, 5894:5900]
        yT = np.empty((B, Dm, S), f32)
        nc = build_ks5(S)
        in_maps = []
        for i in range(NCORES):
            b, hh = divmod(i, 2)
            g0 = 16 * hh
            lay = s5_layout(ssm_a_re[l], ssm_a_im[l], ssm_log_dt[l], ssm_b_re[l], ssm_b_im[l], ssm_c_re[l], ssm_c_im[l], ssm_d[l], g0)
            lay["uT"] = np.ascontiguousarray(u[b, :, g0 * 16:(g0 + 16) * 16].T)
            in_maps.append(lay)
        r = _run(nc, in_maps)
        for i in range(NCORES):
            b, hh = divmod(i, 2)
            yT[b, 256 * hh:256 * (hh + 1), :] = r[i]["yT"]
        del r
        lam_init = 0.8 - 0.6 * math.exp(-0.3 * l)
        nc = build_katt(S, 3, lam_init)
        lamv = np.ascontiguousarray(np.broadcast_to(np.stack([diff_lam_q1[l], diff_lam_k1[l], diff_lam_q2[l], diff_lam_k2[l]]).astype(f32)[None], (128, 4, 64)))
        subln = np.ascontiguousarray(np.broadcast_to(np.asarray(diff_subln[l], f32)[None], (128, 128)))
        in_maps = []
        for i in range(NCORES):
            b, hh = divmod(i, 2)
            h0 = 3 * hh
            qT = np.ascontiguousarray(dq[b, :, h0:h0 + 3].transpose(1, 2, 0))
            kT = np.ascontiguousarray(dk[b, :, h0:h0 + 3].transpose(1, 2, 0))
            in_maps.append({"qT": qT, "qTp": np.ascontiguousarray(qT[:, perm, :]), "kT": kT, "kTp": np.ascontiguousarray(kT[:, perm, :]),
                            "v": np.ascontiguousarray(dv[b, :, h0:h0 + 3]),
                            "pos": np.ascontiguousarray(np.broadcast_to(np.asarray(positions[b], np.int32)[None, :], (128, S))),
                            "invf": invf, "sgn": sgn, "tri": tri, "lamv": lamv, "subln": subln})
        r = _run(nc, in_maps)
        for i in range(NCORES):
            b, hh = divmod(i, 2)
            yT[b, 512 + 384 * hh:512 + 384 * (hh + 1), :] = r[i]["y"].reshape(S, 384).T
        del r
        nc = build_kgdn(S, 3)
        NCH = S // 64
        in_maps = []
        for i in range(NCORES):
            b, hh = divmod(i, 2)
            h0 = 3 * hh
            q3 = gqkv[b].reshape(S, 3, 6, 128)[:, :, h0:h0 + 3]
            pT = np.ascontiguousarray(q3.transpose(1, 2, 3, 0))
            cw = np.ascontiguousarray(np.asarray(gdn_conv[l], f32).reshape(4, 3, 6, 128)[:, :, h0:h0 + 3].transpose(3, 1, 2, 0))
            zt = np.ascontiguousarray(gz[b].reshape(NCH, 64, 6, 128)[:, :, h0:h0 + 3].transpose(1, 2, 0, 3))
            ab = np.ascontiguousarray(np.stack([ga[b], gb[b]], 0).reshape(2, NCH, 64, 6)[:, :, :, h0:h0 + 3].transpose(2, 0, 3, 1))
            hp = np.ascontiguousarray(np.broadcast_to(np.stack([gdn_a_log[l][h0:h0 + 3], gdn_dt_bias[l][h0:h0 + 3]]).astype(f32)[None], (64, 2, 3)))
            gnw = np.ascontiguousarray(np.broadcast_to(np.asarray(gdn_norm[l], f32)[None], (64, 128)))
            in_maps.append({"pT": pT, "cw": cw, "zt": zt, "ab": ab, "hp": hp, "gnw": gnw, "msk": msk, "ident": ident})
        r = _run(nc, in_maps)
        for i in range(NCORES):
            b, hh = divmod(i, 2)
            yT[b, 1280 + 384 * hh:1280 + 384 * (hh + 1), :] = r[i]["y"].reshape(S, 384).T
        del r, proj
        moe = (l % 2 == 1)
        final = (l == 1)
        if not moe:
            nc = build_kb(HALF, 1024, 5632, 1, moe=False, final=final)
            W1 = np.ascontiguousarray(ffn_w1[l // 2][None], dtype=f32)
            W3 = np.ascontiguousarray(ffn_w3[l // 2][None], dtype=f32)
            W2 = np.ascontiguousarray(ffn_w2[l // 2][None], dtype=f32)
        else:
            nc = build_kb(HALF, 1024, 7168, 8, moe=True, final=final)
            W1 = np.ascontiguousarray(moe_w1[l // 2], dtype=f32)
            W3 = np.ascontiguousarray(moe_w3[l // 2], dtype=f32)
            W2 = np.ascontiguousarray(moe_w2[l // 2], dtype=f32)
        wg = np.ascontiguousarray(ssm_w_glu[l], dtype=f32)
        wo = np.ascontiguousarray(w_out[l], dtype=f32)
        in_maps = []
        for i in range(NCORES):
            b, hh = divmod(i, 2)
            tk = slice(hh * HALF, (hh + 1) * HALF)
            vecs = np.ascontiguousarray(np.stack([pscal(v) for v in (norm_ffn[l], scale2[b], shift2[b], gate1[b], gate2[b], norm_final)], axis=1))
            m = {"xT": np.ascontiguousarray(xcur[b, tk].T), "yT": np.ascontiguousarray(yT[b][:, tk]), "wglu": wg, "wout": wo,
                 "vecs": vecs, "w1": W1, "w3": W3, "w2": W2}
            if moe:
                m["wr"] = np.ascontiguousarray(np.asarray(moe_router[l // 2], f32).reshape(16, 128, 8).transpose(1, 0, 2))
                m["sel"] = sel
                m["ident"] = ident
            in_maps.append(m)
        r = _run(nc, in_maps)
        xnew = np.empty((B, S, Dm), f32)
        for i in range(NCORES):
            b, hh = divmod(i, 2)
            xnew[b, hh * HALF:(hh + 1) * HALF] = r[i]["out"].T
        del r
        xcur = xnew
    return xcur
```

```python
import math
import numpy as np
from contextlib import ExitStack
import concourse.bass as bass
import concourse.mybir as mybir
from concourse.bass_utils import run_bass_kernel_spmd

F32 = mybir.dt.float32
BF16 = mybir.dt.bfloat16
I32 = mybir.dt.int32
ALU = mybir.AluOpType
AF = mybir.ActivationFunctionType
AX = mybir.AxisListType


class Prog:
    ENGS = ("pe", "act", "dve", "pool", "sp")

    def __init__(self, nc):
        self.nc = nc
        self.ops = []
        self.stack = ExitStack()

    def sb(self, name, shape, dt):
        return self.stack.enter_context(self.nc.sbuf_tensor(name, list(shape), dt))

    def ps(self, name, shape, dt=F32):
        return self.stack.enter_context(self.nc.psum_tensor(name, list(shape), dt))

    def dram(self, name, shape, dt, kind):
        return self.nc.dram_tensor(name, list(shape), dt, kind=kind).ap()

    def op(self, eng, fn, r=(), w=(), dma=False):
        self.ops.append(dict(eng=eng, fn=fn, r=tuple(r), w=tuple(w), dma=dma))

    def dma(self, out, in_, r=(), w=(), q="sp", **kw):
        self.op(q, lambda e: e.dma_start(out=out, in_=in_, **kw), r=r, w=w, dma=True)

    def emit(self):
        nc = self.nc
        ops = self.ops
        last_w = {}
        dma_w_cnt = {}
        dma_r_cnt = {}
        readers = {}
        prev_readers = {}
        dma_readers_pending = {}
        in_dma_fill = {}
        sig_needed = set()
        waits = [[] for _ in ops]
        for i, o in enumerate(ops):
            need_ops = set()
            need_dma = {}
            def need_writer(k):
                lw = last_w.get(k)
                if lw is None:
                    return
                if lw[0] == "op":
                    need_ops.add(lw[1])
                else:
                    need_dma[("w", k)] = max(need_dma.get(("w", k), 0), lw[1])
            for k in o["r"]:
                need_writer(k)
            for k in o["w"]:
                if o["dma"] and in_dma_fill.get(k):
                    rs = prev_readers.get(k, [])
                else:
                    need_writer(k)
                    rs = readers.get(k, [])
                for j in rs:
                    need_ops.add(j)
                if dma_r_cnt.get(k, 0):
                    need_dma[("r", k)] = dma_r_cnt[k]
            need_ops.discard(i)
            if o["eng"] == "pe" and getattr(self, "pe_inorder", False):
                need_ops = {j for j in need_ops if ops[j]["eng"] != "pe"}
            o["need_ops"] = need_ops
            o["need_dma"] = need_dma
            for j in need_ops:
                sig_needed.add(j)
            for k in o["r"]:
                if o["dma"]:
                    dma_r_cnt[k] = dma_r_cnt.get(k, 0) + 1
                else:
                    readers.setdefault(k, []).append(i)
                in_dma_fill[k] = False
            for k in o["w"]:
                if o["dma"]:
                    if not in_dma_fill.get(k):
                        prev_readers[k] = readers.get(k, [])
                    dma_w_cnt[k] = dma_w_cnt.get(k, 0) + 1
                    last_w[k] = ("dma", dma_w_cnt[k])
                    in_dma_fill[k] = True
                else:
                    last_w[k] = ("op", i)
                    in_dma_fill[k] = False
                readers[k] = []
        dma_keys = set()
        for o in ops:
            if o["dma"]:
                o["incs"] = [("w", k) for k in o["w"]] + [("r", k) for k in o["r"]]
                for s in o["incs"]:
                    dma_keys.add(s)
        sems = {}
        for e in ("pe", "act", "dve", "pool", "sp"):
            sems[e] = self.stack.enter_context(nc.semaphore("s_" + e))
        for n, s in enumerate(sorted(dma_keys, key=str)):
            sems[s] = self.stack.enter_context(nc.semaphore("d%d" % n))
        self.n_sems = len(sems)
        cnt = {e: 0 for e in self.ENGS}
        sigval = {}
        for i, o in enumerate(ops):
            if i in sig_needed and not o["dma"]:
                cnt[o["eng"]] += 1
                sigval[i] = cnt[o["eng"]]
        final_dma = {}
        for o in ops:
            if o["dma"]:
                for s in o["incs"]:
                    final_dma[s] = final_dma.get(s, 0) + 16
        by_eng = {e: [] for e in self.ENGS}
        for i, o in enumerate(ops):
            by_eng[o["eng"]].append(i)
        block = self.stack.enter_context(nc.Block())
        self.n_waits = 0

        def run_engine(ename, eng):
            waited = {}
            for i in by_eng[ename]:
                o = ops[i]
                reqs = {}
                for j in o["need_ops"]:
                    pj = ops[j]
                    if pj["dma"]:
                        continue
                    s = pj["eng"]
                    reqs[s] = max(reqs.get(s, 0), sigval[j])
                for s, n in o["need_dma"].items():
                    reqs[s] = max(reqs.get(s, 0), 16 * n)
                for s, v in reqs.items():
                    if waited.get(s, 0) >= v:
                        continue
                    eng.wait_ge(sems[s], v)
                    waited[s] = v
                    self.n_waits += 1
                ins = o["fn"](eng)
                if o["dma"]:
                    assert len(o["incs"]) >= 1
                    assert len(o["incs"]) == 1, "dma with >1 tracked resource: %s" % (o["incs"],)
                    ins.then_inc(sems[o["incs"][0]], 16)
                elif i in sigval:
                    ins.then_inc(sems[ename], 1)
            if ename == "sp":
                for s, v in final_dma.items():
                    eng.wait_ge(sems[s], v)

        @block.sync
        def _(e):
            run_engine("sp", e)

        @block.tensor
        def _(e):
            run_engine("pe", e)

        @block.scalar
        def _(e):
            run_engine("act", e)

        @block.vector
        def _(e):
            run_engine("dve", e)

        @block.gpsimd
        def _(e):
            run_engine("pool", e)

    def close(self):
        self.stack.close()


def _mm(P, out, lhsT, rhs, r, w, start=True, stop=True, **kw):
    P.op("pe", lambda e: e.matmul(out, lhsT=lhsT, rhs=rhs, start=start, stop=stop, **kw), r=r, w=w)


def _tr(P, out, in_, ident, r, w):
    P.op("pe", lambda e: e.transpose(out, in_, ident), r=r, w=w)


def _act(P, out, in_, func, r, w, eng="act", **kw):
    P.op(eng, lambda e: e.activation(out=out, in_=in_, func=func, **kw), r=r, w=w)


def _ts(P, out, in0, s1, s2, op0, op1, r, w, eng="dve"):
    if s2 is None:
        P.op(eng, lambda e: e.tensor_scalar(out=out, in0=in0, scalar1=s1, scalar2=None, op0=op0), r=r, w=w)
    else:
        P.op(eng, lambda e: e.tensor_scalar(out=out, in0=in0, scalar1=s1, scalar2=s2, op0=op0, op1=op1), r=r, w=w)


def _tt(P, out, in0, in1, op, r, w, eng="dve"):
    P.op(eng, lambda e: e.tensor_tensor(out=out, in0=in0, in1=in1, op=op), r=r, w=w)


def _stt(P, out, in0, scalar, in1, op0, op1, r, w, eng="dve"):
    P.op(eng, lambda e: e.scalar_tensor_tensor(out=out, in0=in0, scalar=scalar, in1=in1, op0=op0, op1=op1), r=r, w=w)


def _cp(P, out, in_, r, w, eng="dve"):
    if eng == "act":
        P.op(eng, lambda e: e.copy(out=out, in_=in_), r=r, w=w)
    else:
        P.op(eng, lambda e: e.tensor_copy(out=out, in_=in_), r=r, w=w)


def _rcp(P, out, in_, r, w):
    P.op("dve", lambda e: e.reciprocal(out=out, in_=in_), r=r, w=w)


D = 2048
KC = 16
EPS = 1e-6


def pscal(v):
    return np.ascontiguousarray(np.asarray(v, np.float32).reshape(KC, 128).T)


def build_k0():
    nc = bass.Bass("TRN2", target_bir_lowering=False)
    P = Prog(nc)
    NCOL = 3072
    cT = P.dram("cT", [128, KC, 4], F32, "ExternalInput")
    w = P.dram("w", [D, NCOL], F32, "ExternalInput")
    bias = P.dram("bias", [4, NCOL], F32, "ExternalInput")
    out = P.dram("out", [4, NCOL], F32, "ExternalOutput")
    c_sb = P.sb("c_sb", [128, KC, 4], F32)
    b_sb = P.sb("b_sb", [4, NCOL], F32)
    o_sb = P.sb("o_sb", [4, NCOL], F32)
    wb = [P.sb("wb%d" % i, [128, KC, 512], F32) for i in range(2)]
    pp = [P.ps("pp%d" % i, [4, 512]) for i in range(2)]
    P.dma(c_sb[:], cT, w=["c"])
    P.dma(b_sb[:], bias, w=["b"])
    P.op("act", lambda e: e.activation(out=c_sb[:], in_=c_sb[:], func=AF.Silu), r=["c"], w=["c"])
    wv = w.rearrange("(kc p) n -> p kc n", p=128)
    for g in range(NCOL // 512):
        s = g % 2
        P.dma(wb[s][:], wv[:, :, g * 512:(g + 1) * 512], w=["wb%d" % s], q=("sp" if s == 0 else "act"))
        for kc in range(KC):
            P.op("pe", lambda e, kc=kc, s=s: e.matmul(pp[s][:], lhsT=c_sb[:, kc, :], rhs=wb[s][:, kc, :],
                                                       start=(kc == 0), stop=(kc == KC - 1)),
                 r=["c", "wb%d" % s], w=["pp%d" % s])
        P.op("dve", lambda e, g=g, s=s: e.tensor_tensor(out=o_sb[:, g * 512:(g + 1) * 512], in0=pp[s][:],
                                                         in1=b_sb[:, g * 512:(g + 1) * 512], op=ALU.add),
             r=["pp%d" % s, "b"], w=["o"])
    P.dma(out, o_sb[:], r=["o"])
    P.emit()
    P.close()
    return nc


def run_k0(c, w_ada, b_ada):
    nc = build_k0()
    cT = np.ascontiguousarray(c.T.reshape(KC, 128, 4).transpose(1, 0, 2))
    in_maps = []
    for i in range(8):
        l, j = divmod(i, 4)
        cols = slice(j * 3072, (j + 1) * 3072)
        in_maps.append({"cT": cT, "w": np.ascontiguousarray(w_ada[l][:, cols]),
                        "bias": np.ascontiguousarray(np.broadcast_to(b_ada[l][cols], (4, 3072)))})
    res = run_bass_kernel_spmd(nc, in_maps, core_ids=list(range(8)))
    mod = np.zeros((2, 4, 12288), np.float32)
    for i in range(8):
        l, j = divmod(i, 4)
        mod[l, :, j * 3072:(j + 1) * 3072] = res.results[i]["out"]
    return mod


def build_ka(NTOK=2048, NOUT=5900):
    nc = bass.Bass("TRN2", target_bir_lowering=False)
    P = Prog(nc)
    NB = NTOK // 128
    x = P.dram("x", [NTOK, D], F32, "ExternalInput")
    w = P.dram("w", [D, NOUT], F32, "ExternalInput")
    vecs = P.dram("vecs", [128, 3, KC], F32, "ExternalInput")
    ident = P.dram("ident", [128, 128], F32, "ExternalInput")
    out = P.dram("out", [NTOK, NOUT], F32, "ExternalOutput")
    v_sb = P.sb("v_sb", [128, 3, KC], F32)
    A_sb = P.sb("A_sb", [128, KC], F32)
    id_sb = P.sb("id_sb", [128, 128], F32)
    hnT = P.sb("hnT", [128, KC, NTOK], BF16)
    xb = [P.sb("xb%d" % i, [128, D], F32) for i in range(2)]
    junk = P.sb("junk", [128, D], F32)
    st = P.sb("st", [128, 4], F32)
    tp = [P.ps("tp%d" % i, [128, 512]) for i in range(2)]
    mp = [P.ps("mp%d" % i, [128, 512]) for i in range(2)]
    wb = [P.sb("wb%d" % i, [128, KC, 512], BF16) for i in range(2)]
    ob = [P.sb("ob%d" % i, [128, 512], F32) for i in range(2)]
    P.dma(v_sb[:], vecs, w=["v"])
    P.dma(id_sb[:], ident, w=["id"])
    P.op("dve", lambda e: e.scalar_tensor_tensor(out=A_sb[:], in0=v_sb[:, 1, :], scalar=1.0, in1=v_sb[:, 0, :],
                                                 op0=ALU.add, op1=ALU.mult), r=["v"], w=["A"])
    for b in range(NB):
        s = b % 2
        P.dma(xb[s][:], x[b * 128:(b + 1) * 128, :], w=["xb%d" % s])
        P.op("act", lambda e, s=s: e.activation(out=junk[:], in_=xb[s][:], func=AF.Square, accum_out=st[:, 0:1]),
             r=["xb%d" % s], w=["junk", "st"])
        P.op("act", lambda e: e.activation(out=st[:, 1:2], in_=st[:, 0:1], func=AF.Sqrt, scale=1.0 / D, bias=st[:, 3:4]),
             r=["st", "eps"], w=["st"])
        P.op("dve", lambda e: e.reciprocal(out=st[:, 2:3], in_=st[:, 1:2]), r=["st"], w=["st"])
        P.op("dve", lambda e, s=s: e.tensor_scalar(out=xb[s][:], in0=xb[s][:], scalar1=st[:, 2:3], scalar2=None,
                                                    op0=ALU.mult), r=["st", "xb%d" % s], w=["xb%d" % s])
        for q in range(4):
            t = (b * 4 + q) % 2
            for j in range(4):
                kc = q * 4 + j
                P.op("pe", lambda e, s=s, t=t, j=j, kc=kc: e.transpose(tp[t][:, j * 128:(j + 1) * 128],
                                                                        xb[s][:, kc * 128:(kc + 1) * 128], id_sb[:]),
                     r=["xb%d" % s, "id"], w=["tp%d" % t])
            for j in range(4):
                kc = q * 4 + j
                P.op("dve" if j % 2 == 0 else "pool" if False else "dve",
                     lambda e, t=t, j=j, kc=kc, b=b: e.tensor_scalar(
                         out=hnT[:, kc, b * 128:(b + 1) * 128], in0=tp[t][:, j * 128:(j + 1) * 128],
                         scalar1=A_sb[:, kc:kc + 1], scalar2=v_sb[:, 2, kc:kc + 1], op0=ALU.mult, op1=ALU.add),
                     r=["tp%d" % t, "A", "v"], w=["hnT%d" % b])
    wv = w.rearrange("(kc p) n -> p kc n", p=128)
    ng = (NOUT + 511) // 512
    cnt = 0
    for g in range(ng):
        c0 = g * 512
        cw = min(512, NOUT - c0)
        s = g % 2
        P.dma(wb[s][:, :, 0:cw], wv[:, :, c0:c0 + cw], w=["wb%d" % s], q="pool")
        for b in range(NB):
            m = cnt % 2
            cnt += 1
            for kc in range(KC):
                P.op("pe", lambda e, m=m, s=s, kc=kc, b=b, cw=cw: e.matmul(
                    mp[m][:, 0:cw], lhsT=hnT[:, kc, b * 128:(b + 1) * 128], rhs=wb[s][:, kc, 0:cw],
                    start=(kc == 0), stop=(kc == KC - 1)), r=["hnT%d" % b, "wb%d" % s], w=["mp%d" % m])
            P.op("act" if m == 0 else "dve",
                 (lambda e, m=m, cw=cw: e.copy(out=ob[m][:, 0:cw], in_=mp[m][:, 0:cw])) if m == 0 else
                 (lambda e, m=m, cw=cw: e.tensor_copy(out=ob[m][:, 0:cw], in_=mp[m][:, 0:cw])),
                 r=["mp%d" % m], w=["ob%d" % m])
            P.dma(out[b * 128:(b + 1) * 128, c0:c0 + cw], ob[m][:, 0:cw], r=["ob%d" % m], q=("sp" if m == 0 else "act"))
    P.ops.insert(0, dict(eng="pool", fn=lambda e: e.memset(st[:, 3:4], EPS), r=(), w=("eps",), dma=False))
    P.emit()
    P.close()
    return nc


import math

PI_SAFE = 3.141592
TWO_PI = 2.0 * math.pi


def att_consts():
    invf = np.zeros((128, 1), np.float32)
    sgn = np.ones((128, 1), np.float32)
    for p in range(128):
        d = p % 64
        if d < 16:
            j = d % 8
            invf[p, 0] = np.float32(500000.0) ** np.float32(-(2.0 * j) / 16.0)
            sgn[p, 0] = -1.0 if d < 8 else 1.0
    k = np.arange(128)[:, None]
    q = np.arange(128)[None, :]
    tri = (q >= k).astype(np.float32)
    return invf, sgn, tri


def rope_perm_index():
    perm = np.arange(128)
    for base in (0, 64):
        for d in range(8):
            perm[base + d] = base + d + 8
            perm[base + 8 + d] = base + d
    return perm


def sincos_tables(P, ang, C, S, tmpi, tmpf, keys_in, key_c, key_s, sgn_ap=None):
    def reduce_to(dst, src, shift):
        P.op("dve", lambda e: e.tensor_scalar(out=tmpi, in0=src, scalar1=shift, scalar2=1.0 / TWO_PI,
                                              op0=ALU.add, op1=ALU.mult), r=keys_in, w=["sc_tmpi"])
        P.op("dve", lambda e: e.tensor_copy(out=tmpf, in_=tmpi), r=["sc_tmpi"], w=["sc_tmpf"])
        P.op("dve", lambda e: e.scalar_tensor_tensor(out=tmpf, in0=tmpf, scalar=-TWO_PI, in1=src,
                                                     op0=ALU.mult, op1=ALU.add), r=["sc_tmpf"] + keys_in, w=["sc_tmpf"])
        P.op("dve", lambda e: e.tensor_scalar(out=tmpf, in0=tmpf, scalar1=shift, scalar2=-PI_SAFE,
                                              op0=ALU.add, op1=ALU.max), r=["sc_tmpf"], w=["sc_tmpf"])
        P.op("dve", lambda e: e.tensor_scalar(out=tmpf, in0=tmpf, scalar1=PI_SAFE, scalar2=None,
                                              op0=ALU.min), r=["sc_tmpf"], w=["sc_tmpf"])
        P.op("act", lambda e: e.activation(out=dst, in_=tmpf, func=AF.Sin), r=["sc_tmpf"], w=[dst_key[0]])
    dst_key = [key_s]
    reduce_to(S, ang, 0.0)
    if sgn_ap is not None:
        P.op("dve", lambda e: e.tensor_scalar(out=S, in0=S, scalar1=sgn_ap, scalar2=None, op0=ALU.mult),
             r=[key_s, "consts"], w=[key_s])
    dst_key[0] = key_c
    reduce_to(C, ang, math.pi / 2.0)


def build_katt(S=4096, NH=3, lambda_init=0.2):
    nc = bass.Bass("TRN2", target_bir_lowering=False)
    P = Prog(nc)
    NBLK = S // 128
    qT = P.dram("qT", [NH, 128, S], F32, "ExternalInput")
    qTp = P.dram("qTp", [NH, 128, S], F32, "ExternalInput")
    kT = P.dram("kT", [NH, 128, S], F32, "ExternalInput")
    kTp = P.dram("kTp", [NH, 128, S], F32, "ExternalInput")
    v = P.dram("v", [S, NH, 128], F32, "ExternalInput")
    pos = P.dram("pos", [128, S], I32, "ExternalInput")
    invf_d = P.dram("invf", [128, 1], F32, "ExternalInput")
    sgn_d = P.dram("sgn", [128, 1], F32, "ExternalInput")
    tri_d = P.dram("tri", [128, 128], F32, "ExternalInput")
    lamv_d = P.dram("lamv", [128, 4, 64], F32, "ExternalInput")
    subln_d = P.dram("subln", [128, 128], F32, "ExternalInput")
    y = P.dram("y", [S, NH, 128], F32, "ExternalOutput")

    invf = P.sb("invf_sb", [128, 1], F32)
    sgn = P.sb("sgn_sb", [128, 1], F32)
    tri = P.sb("tri_sb", [128, 128], BF16)
    lamv = P.sb("lamv_sb", [128, 4, 64], F32)
    subln = P.sb("subln_sb", [128, 128], F32)
    posi = P.sb("posi", [128, S], I32)
    ang = P.sb("ang", [128, S], F32)
    tmpi = P.sb("tmpi", [128, S], I32)
    tmpf = P.sb("tmpf", [128, S], F32)
    Ct = P.sb("Ct", [128, S], F32)
    St = P.sb("St", [128, S], F32)
    stg = [P.sb("stg%d" % i, [128, S], F32) for i in range(2)]
    qb = P.sb("qb", [128, S], BF16)
    kb_ = P.sb("kb", [128, S], BF16)
    vext = P.sb("vext", [128, NBLK, NH, 132], BF16)
    zer = P.sb("zer", [128, 128], BF16)
    lam = P.sb("lam", [128, 8], F32)
    lj = P.sb("lj", [128, 64], F32)
    pT = [P.sb("pT%d" % i, [128, 2, 256], BF16) for i in range(3)]
    sps = [P.ps("sps%d" % i, [128, 2, 256]) for i in range(2)]
    acc = [[P.ps("acc%d_%d" % (i, m), [128, 2, 256]) for m in range(2)] for i in range(2)]
    fin = [P.sb("fin%d" % i, [128, 8], F32) for i in range(2)]
    o0 = [P.sb("o0_%d" % i, [128, 128], F32) for i in range(2)]
    o1 = [P.sb("o1_%d" % i, [128, 128], F32) for i in range(2)]
    yb = [P.sb("yb%d" % i, [128, 128], F32) for i in range(2)]
    epsc = P.sb("epsc", [128, 1], F32)

    P.dma(invf[:], invf_d, w=["consts"])
    P.dma(sgn[:], sgn_d, w=["consts"])
    P.dma(tri[:], tri_d, w=["tri"], q="pool")
    P.dma(lamv[:], lamv_d, w=["lamv"])
    P.dma(subln[:], subln_d, w=["subln"])
    P.dma(posi[:], pos, w=["posi"])
    P.op("pool", lambda e: e.memset(zer[:], 0.0), w=["zer"])
    P.op("pool", lambda e: e.memset(epsc[:], 1e-6), w=["epsc"])
    P.op("pool", lambda e: e.memset(vext[:], 1.0), w=["vext"])
    for t in range(2):
        P.op("dve", lambda e, t=t: e.tensor_tensor(out=lj[:], in0=lamv[:, 2 * t, :], in1=lamv[:, 2 * t + 1, :], op=ALU.mult),
             r=["lamv"], w=["lj"])
        P.op("dve", lambda e, t=t: e.reduce_sum(out=lam[:, t:t + 1], in_=lj[:], axis=AX.X), r=["lj"], w=["lam"])
    P.op("act", lambda e: e.activation(out=lam[:, 2:4], in_=lam[:, 0:2], func=AF.Exp), r=["lam"], w=["lam"])
    P.op("dve", lambda e: e.tensor_tensor(out=lam[:, 4:5], in0=lam[:, 2:3], in1=lam[:, 3:4], op=ALU.subtract), r=["lam"], w=["lam"])
    P.op("dve", lambda e: e.tensor_scalar(out=lam[:, 5:6], in0=lam[:, 4:5], scalar1=float(lambda_init), scalar2=-1.0,
                                          op0=ALU.add, op1=ALU.mult), r=["lam"], w=["lam"])
    P.op("dve", lambda e: e.tensor_scalar(out=subln[:], in0=subln[:], scalar1=float(1.0 - lambda_init), scalar2=None, op0=ALU.mult),
         r=["subln"], w=["subln"])
    P.op("dve", lambda e: e.tensor_copy(out=ang[:], in_=posi[:]), r=["posi"], w=["ang"])
    P.op("dve", lambda e: e.tensor_scalar(out=ang[:], in0=ang[:], scalar1=invf[:, 0:1], scalar2=None, op0=ALU.mult),
         r=["ang", "consts"], w=["ang"])
    sincos_tables(P, ang[:], Ct[:], St[:], tmpi[:], tmpf[:], ["ang"], "Ct", "St", sgn_ap=sgn[:, 0:1])
    vv = v.rearrange("(n p) h d -> p n h d", p=128)
    for h in range(NH):
        P.dma(vext[:, :, h, 0:128], vv[:, :, h, :], w=["vext"], q="pool")

    gi = 0
    for h in range(NH):
        for (src, srcp, dst, dk) in ((qT, qTp, qb, "qb"), (kT, kTp, kb_, "kb")):
            P.dma(stg[0][:], src[h], w=["stg0"])
            P.dma(stg[1][:], srcp[h], w=["stg1"], q="act")
            P.op("dve", lambda e: e.tensor_tensor(out=stg[0][:], in0=stg[0][:], in1=Ct[:], op=ALU.mult), r=["stg0", "Ct"], w=["stg0"])
            P.op("pool", lambda e: e.tensor_tensor(out=stg[1][:], in0=stg[1][:], in1=St[:], op=ALU.mult), r=["stg1", "St"], w=["stg1"])
            P.op("dve", lambda e, dst=dst: e.tensor_tensor(out=dst[:], in0=stg[0][:], in1=stg[1][:], op=ALU.add),
                 r=["stg0", "stg1"], w=[dk])
        NG = S // 256
        for g in range(NG):
            a = gi % 2
            gi += 1
            for m in range(2):
                P.op("pe", lambda e, a=a, m=m: e.matmul(acc[a][m][:].rearrange("p a b -> p (a b)"), lhsT=zer[:], rhs=qb[:, 0:512],
                                                         start=True, stop=False), r=["zer", "qb"], w=["acc%d_%d" % (a, m)])
            nkb = 2 * g + 2
            for kb in range(nkb):
                sidx = kb % 2
                pidx = kb % 3
                diag0 = (kb == 2 * g)
                diag1 = (kb == 2 * g + 1)
                q0 = 256 * g + (128 if diag1 else 0)
                qn = 128 if diag1 else 256
                qo = 128 if diag1 else 0
                for m in range(2):
                    P.op("pe", lambda e, m=m, sidx=sidx, kb=kb, q0=q0, qn=qn, qo=qo: e.matmul(
                        sps[sidx][:, m, qo:qo + qn], lhsT=kb_[m * 64:(m + 1) * 64, kb * 128:(kb + 1) * 128],
                        rhs=qb[m * 64:(m + 1) * 64, q0:q0 + qn], start=True, stop=True),
                        r=["kb", "qb"], w=["sps%d" % sidx])
                P.op("act", lambda e, sidx=sidx, pidx=pidx, qo=qo, qn=qn: e.activation(
                    out=pT[pidx][:, :, qo:qo + qn], in_=sps[sidx][:, :, qo:qo + qn], func=AF.Exp, scale=0.125),
                    r=["sps%d" % sidx], w=["pT%d" % pidx])
                if diag0 or diag1:
                    for m in range(2):
                        P.op("pool", lambda e, m=m, pidx=pidx, qo=qo: e.tensor_tensor(
                            out=pT[pidx][:, m, qo:qo + 128], in0=pT[pidx][:, m, qo:qo + 128], in1=tri[:], op=ALU.mult),
                            r=["pT%d" % pidx, "tri"], w=["pT%d" % pidx])
                for m in range(2):
                    for qs in range(2):
                        if diag1 and qs == 0:
                            continue
                        last = (kb == 2 * g + qs)
                        P.op("pe", lambda e, a=a, m=m, qs=qs, pidx=pidx, kb=kb, h=h, last=last: e.matmul(
                            acc[a][m][:, qs, 0:129], lhsT=pT[pidx][:, m, qs * 128:(qs + 1) * 128], rhs=vext[:, kb, h, 0:129],
                            start=False, stop=last, skip_group_check=True), r=["pT%d" % pidx, "vext"], w=["acc%d_%d" % (a, m)])
            for qs in range(2):
                f = fin[qs]
                blk = 2 * g + qs
                P.op("dve", lambda e, a=a, qs=qs, f=f: e.reciprocal(out=f[:, 0:1], in_=acc[a][0][:, qs, 128:129]), r=["acc%d_0" % a], w=["fin%d" % qs])
                P.op("dve", lambda e, a=a, qs=qs, f=f: e.reciprocal(out=f[:, 1:2], in_=acc[a][1][:, qs, 128:129]), r=["acc%d_1" % a], w=["fin%d" % qs])
                P.op("dve", lambda e, f=f: e.tensor_tensor(out=f[:, 2:3], in0=f[:, 1:2], in1=lam[:, 5:6], op=ALU.mult), r=["fin%d" % qs, "lam"], w=["fin%d" % qs])
                P.op("dve", lambda e, a=a, qs=qs, f=f: e.tensor_scalar(out=o0[qs][:], in0=acc[a][0][:, qs, 0:128], scalar1=f[:, 0:1], scalar2=None, op0=ALU.mult),
                     r=["acc%d_0" % a, "fin%d" % qs], w=["o0_%d" % qs])
                P.op("dve", lambda e, a=a, qs=qs, f=f: e.scalar_tensor_tensor(out=o1[qs][:], in0=acc[a][1][:, qs, 0:128], scalar=f[:, 2:3], in1=o0[qs][:],
                                                                             op0=ALU.mult, op1=ALU.add),
                     r=["acc%d_1" % a, "fin%d" % qs, "o0_%d" % qs], w=["o1_%d" % qs])
                P.op("act", lambda e, qs=qs, f=f: e.activation(out=o0[qs][:], in_=o1[qs][:], func=AF.Square, accum_out=f[:, 3:4]),
                     r=["o1_%d" % qs], w=["o0_%d" % qs, "fin%d" % qs])
                P.op("act", lambda e, f=f: e.activation(out=f[:, 4:5], in_=f[:, 3:4], func=AF.Sqrt, scale=1.0 / 128.0, bias=epsc[:, 0:1]),
                     r=["fin%d" % qs, "epsc"], w=["fin%d" % qs])
                P.op("dve", lambda e, f=f: e.reciprocal(out=f[:, 5:6], in_=f[:, 4:5]), r=["fin%d" % qs], w=["fin%d" % qs])
                P.op("dve", lambda e, qs=qs, f=f: e.scalar_tensor_tensor(out=yb[qs][:], in0=o1[qs][:], scalar=f[:, 5:6], in1=subln[:],
                                                                        op0=ALU.mult, op1=ALU.mult),
                     r=["o1_%d" % qs, "fin%d" % qs, "subln"], w=["yb%d" % qs])
                P.dma(y[blk * 128:(blk + 1) * 128, h, :], yb[qs][:], r=["yb%d" % qs], q=("sp" if qs == 0 else "act"))
    P.emit()
    P.close()
    return nc


import math

NT = 8
CH = 64


def s5_layout(a_re, a_im, log_dt, b_re, b_im, c_re, c_im, d_skip, g0):
    G = 16
    prm = np.zeros((128, 3, NT), np.float32)
    BT = np.zeros((32, 2, NT, 128), np.float32)
    CT = np.zeros((128, 2, NT, 32), np.float32)
    dsk = np.zeros((32, NT), np.float32)
    for i in range(NT):
        for gl in range(2):
            g = g0 + 2 * i + gl
            prm[gl * 64:(gl + 1) * 64, 0, i] = a_re[g]
            prm[gl * 64:(gl + 1) * 64, 1, i] = a_im[g]
            prm[gl * 64:(gl + 1) * 64, 2, i] = log_dt[g]
            BT[gl * 16:(gl + 1) * 16, 0, i, gl * 64:(gl + 1) * 64] = b_re[g].T
            BT[gl * 16:(gl + 1) * 16, 1, i, gl * 64:(gl + 1) * 64] = b_im[g].T
            CT[gl * 64:(gl + 1) * 64, 0, i, gl * 16:(gl + 1) * 16] = c_re[g].T
            CT[gl * 64:(gl + 1) * 64, 1, i, gl * 16:(gl + 1) * 16] = c_im[g].T
            dsk[gl * 16:(gl + 1) * 16, i] = d_skip[g * 16:(g + 1) * 16]
    bidx = np.ascontiguousarray(np.broadcast_to(np.arange(CH + 1, dtype=np.float32)[None], (128, CH + 1)))
    return {"prm": prm, "BT": BT, "CT": CT, "dsk": dsk, "bidx": bidx}


def build_ks5(S=4096):
    nc = bass.Bass("TRN2", target_bir_lowering=False)
    P = Prog(nc)
    TT = 512
    NTT = S // TT
    NCK = TT // CH
    uT = P.dram("uT", [32 * NT, S], F32, "ExternalInput")
    prm_d = P.dram("prm", [128, 3, NT], F32, "ExternalInput")
    BT_d = P.dram("BT", [32, 2, NT, 128], F32, "ExternalInput")
    CT_d = P.dram("CT", [128, 2, NT, 32], F32, "ExternalInput")
    dsk_d = P.dram("dsk", [32, NT], F32, "ExternalInput")
    bidx_d = P.dram("bidx", [128, CH + 1], F32, "ExternalInput")
    yT = P.dram("yT", [32 * NT, S], F32, "ExternalOutput")

    prm = P.sb("prm_sb", [128, 3, NT], F32)
    BT = P.sb("BT_sb", [32, 2, NT, 128], F32)
    CT = P.sb("CT_sb", [128, 2, NT, 32], F32)
    dsk = P.sb("dsk_sb", [32, NT], F32)
    bidx = P.sb("bidx_sb", [128, CH + 1], F32)
    u_bufs = [P.sb("u_sb%d" % i, [32, NT, 512], F32) for i in range(2)]
    sc = P.sb("sc", [128, 24, NT], F32)
    ang = P.sb("ang", [128, NT, CH + 2], F32)
    tmpi = P.sb("tmpi", [128, NT, CH + 2], I32)
    tmpf = P.sb("tmpf", [128, NT, CH + 2], F32)
    cosb = P.sb("cosb", [128, NT, CH + 2], F32)
    sinb = P.sb("sinb", [128, NT, CH + 2], F32)
    nsinb = P.sb("nsinb", [128, NT, CH + 2], F32)
    tin_re = P.sb("tin_re", [128, NT, CH], F32)
    tin_im = P.sb("tin_im", [128, NT, CH], F32)
    ttmp = P.sb("ttmp", [128, NT, CH], F32)
    rtab = P.sb("rtab", [128, NT, CH], F32)
    in_re = P.sb("in_re", [128, TT], F32)
    in_im = P.sb("in_im", [128, TT], F32)
    t1 = P.sb("t1", [128, TT], F32)
    t2 = P.sb("t2", [128, TT], F32)
    z_re = P.sb("z_re", [128, NT, TT], F32)
    z_im = P.sb("z_im", [128, NT, TT], F32)
    x_re = P.sb("x_re", [128, TT], F32)
    x_im = P.sb("x_im", [128, TT], F32)
    t3 = P.sb("t3", [128, TT], F32)
    t4 = P.sb("t4", [128, TT], F32)
    init = P.sb("init", [128, 2, NT], F32)
    it = P.sb("it", [128, 4, NT], F32)
    ysb = [P.sb("ysb%d" % i, [32, TT], F32) for i in range(2)]
    p_re = P.ps("p_re", [128, TT])
    p_im = P.ps("p_im", [128, TT])
    p_y = [P.ps("p_y%d" % i, [32, TT]) for i in range(2)]

    P.dma(prm[:], prm_d, w=["prm"])
    P.dma(BT[:], BT_d, w=["BT"])
    P.dma(CT[:], CT_d, w=["CT"])
    P.dma(dsk[:], dsk_d, w=["dsk"])
    P.dma(bidx[:], bidx_d, w=["bidx"])
    a_re, a_im = prm[:, 0, :], prm[:, 1, :]
    dt, r_, th = sc[:, 0, :], sc[:, 1, :], sc[:, 2, :]
    K = ["sc"]
    _act(P, dt, prm[:, 2, :], AF.Exp, r=["prm"], w=K)
    _tt(P, th, a_im, dt, ALU.mult, r=["prm"] + K, w=K)
    _tt(P, r_, a_re, dt, ALU.mult, r=["prm"] + K, w=K)
    _act(P, r_, r_, AF.Exp, r=K, w=K)
    for i in range(NT):
        _ts(P, ang[:, i, 0:CH + 1], bidx[:], sc[:, 2, i:i + 1], None, ALU.mult, None, r=["bidx"] + K, w=["ang"])
    _cp(P, ang[:, :, CH + 1], th, r=K, w=["ang"])
    af = lambda t: t[:].rearrange("p a b -> p (a b)")
    sincos_tables(P, af(ang), af(cosb), af(sinb), af(tmpi), af(tmpf), ["ang"], "cosb", "sinb")
    _ts(P, af(nsinb), af(sinb), -1.0, None, ALU.mult, None, r=["sinb"], w=["nsinb"])
    cth, sth = cosb[:, :, CH + 1], sinb[:, :, CH + 1]
    nr, ni, den, cr, ci, tA, tB = (sc[:, j, :] for j in range(3, 10))
    _tt(P, nr, r_, cth, ALU.mult, r=K + ["cosb"], w=K)
    _ts(P, nr, nr, -1.0, None, ALU.add, None, r=K, w=K)
    _tt(P, ni, r_, sth, ALU.mult, r=K + ["sinb"], w=K)
    _tt(P, den, a_re, a_re, ALU.mult, r=["prm"], w=K)
    _tt(P, tA, a_im, a_im, ALU.mult, r=["prm"], w=K)
    _tt(P, den, den, tA, ALU.add, r=K, w=K)
    _rcp(P, den, den, r=K, w=K)
    _tt(P, cr, nr, a_re, ALU.mult, r=K + ["prm"], w=K)
    _tt(P, tA, ni, a_im, ALU.mult, r=K + ["prm"], w=K)
    _tt(P, cr, cr, tA, ALU.add, r=K, w=K)
    _tt(P, cr, cr, den, ALU.mult, r=K, w=K)
    _tt(P, ci, ni, a_re, ALU.mult, r=K + ["prm"], w=K)
    _tt(P, tA, nr, a_im, ALU.mult, r=K + ["prm"], w=K)
    _tt(P, ci, ci, tA, ALU.subtract, r=K, w=K)
    _tt(P, ci, ci, den, ALU.mult, r=K, w=K)
    for i in range(NT):
        _ts(P, tin_re[:, i, :], cosb[:, i, 0:CH], sc[:, 6, i:i + 1], None, ALU.mult, None, r=K + ["cosb"], w=["tin_re"])
        _stt(P, tin_re[:, i, :], sinb[:, i, 0:CH], sc[:, 7, i:i + 1], tin_re[:, i, :], ALU.mult, ALU.add, r=K + ["sinb", "tin_re"], w=["tin_re"])
        _ts(P, tin_im[:, i, :], cosb[:, i, 0:CH], sc[:, 7, i:i + 1], None, ALU.mult, None, r=K + ["cosb"], w=["tin_im"])
        _ts(P, ttmp[:, i, :], sinb[:, i, 0:CH], sc[:, 6, i:i + 1], None, ALU.mult, None, r=K + ["sinb"], w=["ttmp"])
        _tt(P, tin_im[:, i, :], tin_im[:, i, :], ttmp[:, i, :], ALU.subtract, r=["tin_im", "ttmp"], w=["tin_im"])
        P.op("pool", lambda e, i=i: e.memset(rtab[:, i, :], 1.0), w=["rtab"])
        _ts(P, rtab[:, i, :], rtab[:, i, :], sc[:, 1, i:i + 1], None, ALU.mult, None, r=K + ["rtab"], w=["rtab"])
    c64, s64 = cosb[:, :, CH], sinb[:, :, CH]
    P.op("pool", lambda e: e.memset(init[:], 0.0), w=["init%d" % i for i in range(NT)])

    def bc(t, i):
        return t[:, i:i + 1, :].to_broadcast([128, NCK, CH])

    def v3(t):
        return t[:].rearrange("p (a b) -> p a b", b=CH)

    yi = 0
    for tt in range(NTT):
        tok = slice(tt * TT, (tt + 1) * TT)
        u_sb = u_bufs[tt % 2]
        uk = "u%d" % (tt % 2)
        P.dma(u_sb[:], uT.rearrange("(i c) s -> c i s", c=32)[:, :, tok], w=[uk], q="act")
        for i in range(NT):
            _mm(P, p_re[:], BT[:, 0, i, :], u_sb[:, i, :], r=["BT", uk], w=["p_re"])
            _mm(P, p_im[:], BT[:, 1, i, :], u_sb[:, i, :], r=["BT", uk], w=["p_im"])
            pr3 = p_re[:].rearrange("p (a b) -> p a b", b=CH)
            pi3 = p_im[:].rearrange("p (a b) -> p a b", b=CH)
            _tt(P, v3(in_re), pr3, bc(tin_re, i), ALU.mult, r=["p_re", "tin_re"], w=["in_re"])
            _tt(P, v3(t1), pi3, bc(tin_im, i), ALU.mult, r=["p_im", "tin_im"], w=["t1"])
            _tt(P, v3(in_im), pr3, bc(tin_im, i), ALU.mult, r=["p_re", "tin_im"], w=["in_im"])
            _tt(P, v3(t2), pi3, bc(tin_re, i), ALU.mult, r=["p_im", "tin_re"], w=["t2"])
            _tt(P, in_re[:], in_re[:], t1[:], ALU.subtract, r=["in_re", "t1"], w=["in_re"], eng="pool")
            _tt(P, in_im[:], in_im[:], t2[:], ALU.add, r=["in_im", "t2"], w=["in_im"], eng="pool")
            for a in range(NCK):
                cs = slice(a * CH, (a + 1) * CH)
                P.op("dve", lambda e, i=i, cs=cs: e.tensor_tensor_scan(out=z_re[:, i, cs], data0=rtab[:, i, :], data1=in_re[:, cs],
                                                                       initial=init[:, 0, i:i + 1], op0=ALU.mult, op1=ALU.add),
                     r=["rtab", "in_re", "init%d" % i], w=["z_re%d" % i])
                P.op("dve", lambda e, i=i, cs=cs: e.tensor_tensor_scan(out=z_im[:, i, cs], data0=rtab[:, i, :], data1=in_im[:, cs],
                                                                       initial=init[:, 1, i:i + 1], op0=ALU.mult, op1=ALU.add),
                     r=["rtab", "in_im", "init%d" % i], w=["z_im%d" % i])
                last = a * CH + CH - 1
                zr, zi = z_re[:, i, last:last + 1], z_im[:, i, last:last + 1]
                kk = ["z_re%d" % i, "z_im%d" % i, "cosb", "sinb"]
                _ts(P, it[:, 0, i:i + 1], zr, c64[:, i:i + 1], None, ALU.mult, None, r=kk, w=["it%d" % i])
                _ts(P, it[:, 1, i:i + 1], zr, s64[:, i:i + 1], None, ALU.mult, None, r=kk, w=["it%d" % i])
                _stt(P, init[:, 0, i:i + 1], zi, nsinb[:, i, CH:CH + 1], it[:, 0, i:i + 1], ALU.mult, ALU.add, r=kk + ["nsinb", "it%d" % i], w=["init%d" % i])
                _stt(P, init[:, 1, i:i + 1], zi, c64[:, i:i + 1], it[:, 1, i:i + 1], ALU.mult, ALU.add, r=kk + ["it%d" % i], w=["init%d" % i])
            zr3 = z_re[:, i, :].rearrange("p (a b) -> p a b", b=CH)
            zi3 = z_im[:, i, :].rearrange("p (a b) -> p a b", b=CH)
            cb = cosb[:, i:i + 1, 0:CH].to_broadcast([128, NCK, CH])
            sb_ = sinb[:, i:i + 1, 0:CH].to_broadcast([128, NCK, CH])
            nsb = nsinb[:, i:i + 1, 0:CH].to_broadcast([128, NCK, CH])
            _tt(P, v3(x_re), zr3, cb, ALU.mult, r=["z_re%d" % i, "cosb"], w=["x_re"], eng="pool")
            _tt(P, v3(t3), zi3, sb_, ALU.mult, r=["z_im%d" % i, "sinb"], w=["t3"], eng="pool")
            _tt(P, x_re[:], x_re[:], t3[:], ALU.subtract, r=["x_re", "t3"], w=["x_re"], eng="pool")
            _tt(P, v3(x_im), zr3, nsb, ALU.mult, r=["z_re%d" % i, "nsinb"], w=["x_im"], eng="pool")
            _tt(P, v3(t4), zi3, cb, ALU.mult, r=["z_im%d" % i, "cosb"], w=["t4"], eng="pool")
            _tt(P, x_im[:], x_im[:], t4[:], ALU.subtract, r=["x_im", "t4"], w=["x_im"], eng="pool")
            k = yi % 2
            yi += 1
            _mm(P, p_y[k][:], CT[:, 0, i, :], x_re[:], r=["CT", "x_re"], w=["p_y%d" % k], start=True, stop=False)
            _mm(P, p_y[k][:], CT[:, 1, i, :], x_im[:], r=["CT", "x_im"], w=["p_y%d" % k], start=False, stop=True)
            _stt(P, ysb[k][:], u_sb[:, i, :], dsk[:, i:i + 1], p_y[k][:], ALU.mult, ALU.add, r=[uk, "dsk", "p_y%d" % k], w=["ysb%d" % k])
            P.dma(yT[32 * i:32 * (i + 1), tok], ysb[k][:], r=["ysb%d" % k], q="sp")
    P.emit()
    P.close()
    return nc


C = 64


def gdn_consts():
    r = np.arange(C)[:, None]
    c = np.arange(C)[None, :]
    upi = (r <= c).astype(np.float32)
    los = (r > c).astype(np.float32)
    loi = (r >= c).astype(np.float32)
    return np.ascontiguousarray(np.stack([upi, los, loi], axis=1))


def build_kgdn(S=4096, NH=3):
    nc = bass.Bass("TRN2", target_bir_lowering=False)
    P = Prog(nc)
    NCH = S // C
    NB = 8
    pT = P.dram("pT", [3, NH, 128, S], F32, "ExternalInput")
    cw_d = P.dram("cw", [128, 3, NH, 4], F32, "ExternalInput")
    zt_d = P.dram("zt", [C, NH, NCH, 128], F32, "ExternalInput")
    ab_d = P.dram("ab", [C, 2, NH, NCH], F32, "ExternalInput")
    hp_d = P.dram("hp", [C, 2, NH], F32, "ExternalInput")
    gnw_d = P.dram("gnw", [C, 128], F32, "ExternalInput")
    msk_d = P.dram("msk", [C, 3, C], F32, "ExternalInput")
    id_d = P.dram("ident", [128, 128], F32, "ExternalInput")
    y = P.dram("y", [S, NH, 128], F32, "ExternalOutput")

    cw = P.sb("cw_sb", [128, 3, NH, 4], F32)
    ab = P.sb("ab_sb", [C, 2, NH, NCH], F32)
    hp = P.sb("hp_sb", [C, 2, NH], F32)
    gnw = P.sb("gnw_sb", [C, 128], F32)
    msk = P.sb("msk_sb", [C, 3, C], F32)
    ident = P.sb("id_sb", [128, 128], F32)
    ones = P.sb("ones", [C, 128], F32)
    epsc = P.sb("epsc", [128, 1], F32)
    g = P.sb("g", [C, NH, NCH], F32)
    beta = P.sb("beta", [C, NH, NCH], F32)
    egc = P.sb("egc", [C, NH, NCH], F32)
    gcs = P.sb("gcs", [C, NH * NCH], F32)
    edec = P.sb("edec", [C, NH, NCH], F32)
    egl = P.sb("egl", [128, NH, NCH], F32)
    raw = P.sb("raw", [128, S + 4], F32)
    cv = [P.sb("cv%d" % i, [128, S], F32) for i in range(3)]
    zt = P.sb("zt_sb", [C, NB, 128], F32)
    qkv = P.sb("qkv", [C, NB, 3, 128], F32)
    aux = P.sb("aux", [C, NB, 3, 128], F32)
    st = P.sb("st", [C, NB, 8], F32)
    junk = P.sb("junk", [C, 128], F32)
    knT = P.sb("knT", [128, NB, C], F32)
    qnT = P.sb("qnT", [128, NB, C], F32)
    qgT = P.sb("qgT", [128, NB, C], F32)
    wT = P.sb("wT", [128, NB, C], F32)
    G2 = P.sb("G2", [C, NB, C], F32)
    E = P.sb("E", [C, NB, C], F32)
    ET = P.sb("ET", [C, NB, C], F32)
    Lm = P.sb("Lm", [C, NB, C], F32)
    Am = P.sb("Am", [C, NB, C], F32)
    Pm = [P.sb("Pm%d" % i, [C, NB, C], F32) for i in range(2)]
    Qm = [P.sb("Qm%d" % i, [C, NB, C], F32) for i in range(2)]
    X = P.sb("X", [C, NB, C], F32)
    QKT = P.sb("QKT", [C, NB, C], F32)
    uw = P.sb("uw", [C, NB, 256], F32)
    Sst = P.sb("Sst", [128, 128], F32)
    vnew = P.sb("vnew", [C, 128], F32)
    osb = [P.sb("osb%d" % i, [C, 128], F32) for i in range(2)]
    ost = P.sb("ost", [C, 8], F32)
    zs = P.sb("zs", [C, NB, 128], F32)
    pa = P.ps("pa", [128, 512])
    pb = P.ps("pb", [128, 512])
    pc = P.ps("pc", [128, 512])
    pd = P.ps("pd", [128, 512])
    pws = P.ps("pws", [C, 128])
    po = P.ps("po", [C, 128])
    pds = P.ps("pds", [128, 128])
    pg = P.ps("pg", [128, 512])

    P.dma(cw[:], cw_d, w=["cw"])
    P.dma(ab[:], ab_d, w=["ab"])
    P.dma(hp[:], hp_d, w=["hp"])
    P.dma(gnw[:], gnw_d, w=["gnw"])
    P.dma(msk[:], msk_d, w=["msk"])
    P.dma(ident[:], id_d, w=["ident"])
    P.op("pool", lambda e: e.memset(ones[:], 1.0), w=["ones"])
    P.op("pool", lambda e: e.memset(epsc[:], 1e-6), w=["epsc"])
    UPI, LOS, LOI = msk[:, 0, :], msk[:, 1, :], msk[:, 2, :]

    _act(P, hp[:, 0, :], hp[:, 0, :], AF.Exp, r=["hp"], w=["hp"])
    for h in range(NH):
        _act(P, g[:, h, :], ab[:, 0, h, :], AF.Exp, r=["ab", "hp"], w=["g"], bias=hp[:, 1, h:h + 1])
        _act(P, g[:, h, :], g[:, h, :], AF.Ln, r=["g"], w=["g"], bias=1.0)
        _ts(P, g[:, h, :], g[:, h, :], hp[:, 0, h:h + 1], -1.0, ALU.mult, ALU.mult, r=["g", "hp"], w=["g"])
    _act(P, beta[:].rearrange("p h n -> p (h n)"), ab[:, 1, :, :].rearrange("p h n -> p (h n)"), AF.Sigmoid, r=["ab"], w=["beta"])
    NF = NH * NCH
    gf = g[:].rearrange("p h n -> p (h n)")
    _mm(P, pg[0:C, 0:NF], UPI, gf, r=["msk", "g"], w=["pg"])
    _mm(P, pg[0:C, 256:256 + NF], ones[:, 0:C], gf, r=["ones", "g"], w=["pg"])
    _mm(P, pb[:, 0:NF], ones[:, :], gf, r=["ones", "g"], w=["pb"])
    _cp(P, gcs[:], pg[0:C, 0:NF], r=["pg"], w=["gcs"])
    _act(P, egc[:].rearrange("p h n -> p (h n)"), gcs[:], AF.Exp, r=["gcs"], w=["egc"])
    _tt(P, edec[:].rearrange("p h n -> p (h n)"), pg[0:C, 256:256 + NF], gcs[:], ALU.subtract, r=["pg", "gcs"], w=["edec"])
    _act(P, edec[:].rearrange("p h n -> p (h n)"), edec[:].rearrange("p h n -> p (h n)"), AF.Exp, r=["edec"], w=["edec"])
    _act(P, egl[:].rearrange("p h n -> p (h n)"), pb[:, 0:NF], AF.Exp, r=["pb"], w=["egl"])

    for h in range(NH):
        for t in range(3):
            P.op("pool", lambda e: e.memset(raw[:, 0:4], 0.0), w=["raw"])
            P.dma(raw[:, 4:4 + S], pT[t, h], w=["raw"])
            _ts(P, cv[t][:], raw[:, 4:4 + S], cw[:, t, h, 3:4], None, ALU.mult, None, r=["raw", "cw"], w=["cv%d" % t])
            for j in range(3):
                _stt(P, cv[t][:], raw[:, 1 + j:1 + j + S], cw[:, t, h, j:j + 1], cv[t][:], ALU.mult, ALU.add,
                     r=["raw", "cw", "cv%d" % t], w=["cv%d" % t], eng="dve")
            _act(P, cv[t][:], cv[t][:], AF.Silu, r=["cv%d" % t], w=["cv%d" % t])
        P.op("pool", lambda e: e.memset(Sst[:], 0.0), w=["S"])
        for b0 in range(0, NCH, NB):
            P.dma(zt[:], zt_d[:, h, b0:b0 + NB, :], w=["zt"], q="act")
            for i in range(NB):
                n = b0 + i
                ps_t = (pa, pb)[i % 2]
                kt = "pa" if i % 2 == 0 else "pb"
                for t in range(3):
                    _tr(P, ps_t[0:C, t * 128:(t + 1) * 128], cv[t][:, n * C:(n + 1) * C], ident[:], r=["cv%d" % t, "ident"], w=[kt])
                for t in range(2):
                    _act(P, junk[:], ps_t[0:C, t * 128:(t + 1) * 128], AF.Square, r=[kt], w=["junk", "st"], accum_out=st[:, i, t:t + 1])
                _act(P, st[:, i, 2:4], st[:, i, 0:2], AF.Sqrt, r=["st", "epsc"], w=["st"], bias=epsc[0:C, 0:1])
                _rcp(P, st[:, i, 4:6], st[:, i, 2:4], r=["st"], w=["st"])
                _ts(P, qkv[:, i, 0, :], ps_t[0:C, 0:128], st[:, i, 4:5], float(128 ** -0.5), ALU.mult, ALU.mult, r=[kt, "st"], w=["qkv"])
                _ts(P, qkv[:, i, 1, :], ps_t[0:C, 128:256], st[:, i, 5:6], None, ALU.mult, None, r=[kt, "st"], w=["qkv"])
                _ts(P, qkv[:, i, 2, :], ps_t[0:C, 256:384], beta[:, h, n:n + 1], None, ALU.mult, None, r=[kt, "beta"], w=["qkv"])
                _ts(P, aux[:, i, 0, :], qkv[:, i, 1, :], beta[:, h, n:n + 1], egc[:, h, n:n + 1], ALU.mult, ALU.mult, r=["qkv", "beta", "egc"], w=["aux"], eng="pool")
                _ts(P, aux[:, i, 1, :], qkv[:, i, 0, :], egc[:, h, n:n + 1], None, ALU.mult, None, r=["qkv", "egc"], w=["aux"], eng="pool")
                _ts(P, aux[:, i, 2, :], qkv[:, i, 1, :], edec[:, h, n:n + 1], None, ALU.mult, None, r=["qkv", "edec"], w=["aux"], eng="pool")
                _ts(P, G2[:, i, :], LOS, g[:, h, n:n + 1], None, ALU.mult, None, r=["msk", "g"], w=["G2"], eng="pool")
            for (src_t, src_j, dst, dk, pst, pk) in ((qkv, 1, knT, "knT", pc, "pc"), (qkv, 0, qnT, "qnT", pd, "pd"), (aux, 1, qgT, "qgT", pc, "pc")):
                srck = "qkv" if src_t is qkv else "aux"
                for i in range(NB):
                    _tr(P, pst[:, i * C:(i + 1) * C], src_t[:, i, src_j, :], ident[0:C, 0:C], r=[srck, "ident"], w=[pk])
                _cp(P, dst[:].rearrange("p a b -> p (a b)"), pst[:, 0:NB * C], r=[pk], w=[dk], eng=("act" if dk == "qnT" else "dve"))
            for i in range(NB):
                _mm(P, pa[0:C, i * C:(i + 1) * C], UPI, G2[:, i, :], r=["msk", "G2"], w=["pa"])
            for i in range(NB):
                _mm(P, pb[0:C, i * C:(i + 1) * C], G2[:, i, :], UPI, r=["msk", "G2"], w=["pb"])
            _act(P, E[:].rearrange("p a b -> p (a b)"), pa[0:C, 0:NB * C], AF.Exp, r=["pa"], w=["E"])
            _act(P, ET[:].rearrange("p a b -> p (a b)"), pb[0:C, 0:NB * C], AF.Exp, r=["pb"], w=["ET"])
            for i in range(NB):
                _mm(P, pc[0:C, i * C:(i + 1) * C], knT[:, i, :], knT[:, i, :], r=["knT"], w=["pc"])
            for i in range(NB):
                _mm(P, pd[0:C, i * C:(i + 1) * C], knT[:, i, :], qnT[:, i, :], r=["knT", "qnT"], w=["pd"])
            for i in range(NB):
                n = b0 + i
                _stt(P, Lm[:, i, :], pc[0:C, i * C:(i + 1) * C], beta[:, h, n:n + 1], E[:, i, :], ALU.mult, ALU.mult, r=["pc", "beta", "E"], w=["Lm"])
                _tt(P, Lm[:, i, :], Lm[:, i, :], LOS, ALU.mult, r=["Lm", "msk"], w=["Lm"], eng="pool")
                _tt(P, QKT[:, i, :], pd[0:C, i * C:(i + 1) * C], ET[:, i, :], ALU.mult, r=["pd", "ET"], w=["QKT"])
                _tt(P, QKT[:, i, :], QKT[:, i, :], UPI, ALU.mult, r=["QKT", "msk"], w=["QKT"], eng="pool")
            for i in range(NB):
                _tr(P, pa[0:C, i * C:(i + 1) * C], Lm[:, i, :], ident[0:C, 0:C], r=["Lm", "ident"], w=["pa"])
            _cp(P, Am[:].rearrange("p a b -> p (a b)"), pa[0:C, 0:NB * C], r=["pa"], w=["Am"])
            for i in range(NB):
                _tt(P, X[:, i, :], ident[0:C, 0:C], Am[:, i, :], ALU.subtract, r=["ident", "Am"], w=["X"], eng="pool")
            Pc, Qc, pk_, qk_ = Am, Lm, "Am", "Lm"
            for lvl in range(5):
                Pn, Qn = Pm[lvl % 2], Qm[lvl % 2]
                pnk, qnk = "Pm%d" % (lvl % 2), "Qm%d" % (lvl % 2)
                for i in range(NB):
                    _mm(P, pa[0:C, i * C:(i + 1) * C], Qc[:, i, :], Pc[:, i, :], r=[pk_, qk_], w=["pa"])
                for i in range(NB):
                    _mm(P, pb[0:C, i * C:(i + 1) * C], Pc[:, i, :], Qc[:, i, :], r=[pk_, qk_], w=["pb"])
                _cp(P, Pn[:].rearrange("p a b -> p (a b)"), pa[0:C, 0:NB * C], r=["pa"], w=[pnk])
                _cp(P, Qn[:].rearrange("p a b -> p (a b)"), pb[0:C, 0:NB * C], r=["pb"], w=[qnk], eng="act")
                for i in range(NB):
                    _mm(P, pc[0:C, i * C:(i + 1) * C], Qn[:, i, :], X[:, i, :], r=[qnk, "X"], w=["pc"])
                _tt(P, X[:].rearrange("p a b -> p (a b)"), X[:].rearrange("p a b -> p (a b)"), pc[0:C, 0:NB * C], ALU.add, r=["X", "pc"], w=["X"])
                Pc, Qc, pk_, qk_ = Pn, Qn, pnk, qnk
            for i in range(NB):
                pst, pk = ((pa, "pa"), (pb, "pb"))[(i // 2) % 2]
                _mm(P, pst[0:C, (i % 2) * 256:(i % 2) * 256 + 128], X[:, i, :], qkv[:, i, 2, :], r=["X", "qkv"], w=[pk])
                _mm(P, pst[0:C, (i % 2) * 256 + 128:(i % 2) * 256 + 256], X[:, i, :], aux[:, i, 0, :], r=["X", "aux"], w=[pk])
                if i % 2 == 1:
                    _cp(P, uw[:, i - 1:i + 1, :].rearrange("p a b -> p (a b)"), pst[0:C, 0:512], r=[pk], w=["uw"], eng=("dve" if (i // 2) % 2 == 0 else "act"))
            for i in range(NB):
                _tr(P, pc[:, i * C:(i + 1) * C], uw[:, i, 128:256], ident[0:C, 0:C], r=["uw", "ident"], w=["pc"])
            _cp(P, wT[:].rearrange("p a b -> p (a b)"), pc[:, 0:NB * C], r=["pc"], w=["wT"])
            _act(P, zs[:].rearrange("p a b -> p (a b)"), zt[:].rearrange("p a b -> p (a b)"), AF.Silu, r=["zt"], w=["zs"])
            for i in range(NB):
                n = b0 + i
                ob = osb[i % 2]
                obk = "osb%d" % (i % 2)
                _mm(P, pws[:], wT[:, i, :], Sst[:], r=["wT", "S"], w=["pws"])
                _tt(P, vnew[:], uw[:, i, 0:128], pws[:], ALU.subtract, r=["uw", "pws"], w=["vnew"])
                _mm(P, po[:], qgT[:, i, :], Sst[:], r=["qgT", "S"], w=["po"], start=True, stop=False)
                _mm(P, po[:], QKT[:, i, :], vnew[:], r=["QKT", "vnew"], w=["po"], start=False, stop=True)
                _mm(P, pds[:], aux[:, i, 2, :], vnew[:], r=["aux", "vnew"], w=["pds"])
                _stt(P, Sst[:], Sst[:], egl[:, h, n:n + 1], pds[:], ALU.mult, ALU.add, r=["S", "egl", "pds"], w=["S"])
                _act(P, junk[:], po[:], AF.Square, r=["po"], w=["junk", "ost"], accum_out=ost[:, 0:1])
                _act(P, ost[:, 1:2], ost[:, 0:1], AF.Sqrt, r=["ost", "epsc"], w=["ost"], scale=1.0 / 128.0, bias=epsc[0:C, 0:1])
                _rcp(P, ost[:, 2:3], ost[:, 1:2], r=["ost"], w=["ost"])
                _stt(P, ob[:], po[:], ost[:, 2:3], gnw[:], ALU.mult, ALU.mult, r=["po", "ost", "gnw"], w=[obk])
                _tt(P, ob[:], ob[:], zs[:, i, :], ALU.mult, r=[obk, "zs"], w=[obk], eng="pool")
                P.dma(y[n * C:(n + 1) * C, h, :], ob[:], r=[obk], q="sp")
    P.emit()
    P.close()
    return nc


D = 2048
KC = 16


def build_kb(NTOK=2048, NPASS=1024, DFF=5632, NE=1, moe=False, final=False):
    nc = bass.Bass("TRN2", target_bir_lowering=False)
    P = Prog(nc)
    P.pe_inorder = True
    NP = NPASS
    NH = NP // 512
    xT = P.dram("xT", [D, NTOK], F32, "ExternalInput")
    yT = P.dram("yT", [D, NTOK], F32, "ExternalInput")
    wglu = P.dram("wglu", [512, 512], F32, "ExternalInput")
    wout = P.dram("wout", [D, D], F32, "ExternalInput")
    vecs_d = P.dram("vecs", [128, 6, KC], F32, "ExternalInput")
    w1 = P.dram("w1", [NE, D, DFF], F32, "ExternalInput")
    w3 = P.dram("w3", [NE, D, DFF], F32, "ExternalInput")
    w2 = P.dram("w2", [NE, DFF, D], F32, "ExternalInput")
    if moe:
        wr_d = P.dram("wr", [128, KC, 8], F32, "ExternalInput")
        sel_d = P.dram("sel", [8, 8, 128], F32, "ExternalInput")
        id_d = P.dram("ident", [128, 128], F32, "ExternalInput")
    out = P.dram("out", [D, NTOK], F32, "ExternalOutput")

    vecs = P.sb("vecs_sb", [128, 6, KC], F32)
    A2 = P.sb("A2", [128, KC], F32)
    ones = P.sb("ones", [128, 128], F32)
    epsc = P.sb("epsc", [128, 1], F32)
    x1T = P.sb("x1T", [128, KC, NP], F32)
    bufA = P.sb("bufA", [128, KC, NP], BF16)
    ysf = P.sb("ysf", [128, 4, 512], F32)
    ysg = P.sb("ysg", [128, 4, 512], F32)
    wg_sb = P.sb("wg_sb", [128, 4, 512], BF16)
    WA = [P.sb("WA%d" % i, [128, KC, 256], BF16) for i in range(2)]
    WB = [P.sb("WB%d" % i, [128, KC, 256], BF16) for i in range(2)]
    WC = [P.sb("WC%d" % i, [128, 2, D], BF16) for i in range(2)]
    s_sb = [P.sb("s_sb%d" % i, [128, 512], BF16) for i in range(2)]
    a_sb = [P.sb("a_sb%d" % i, [128, 2, 512], BF16) for i in range(2)]
    sq = [P.sb("sq%d" % i, [128, 512], F32) for i in range(2)]
    rstd = P.sb("rstd", [128, 512], F32)
    ph1 = [P.ps("ph1_%d" % i, [128, 512]) for i in range(2)]
    ph3 = [P.ps("ph3_%d" % i, [128, 512]) for i in range(2)]
    po = [P.ps("po%d" % i, [128, 512]) for i in range(2)]
    pn = [P.ps("pn%d" % i, [128, 512]) for i in range(2)]
    if moe:
        wr = P.sb("wr_sb", [128, KC, 8], F32)
        sel = P.sb("sel_sb", [8, 8, 128], F32)
        ident = P.sb("id_sb", [128, 128], F32)
        comb = P.sb("comb", [128, 8, NP], BF16)
        lgT = P.sb("lgT", [8, 512], F32)
        lg = P.sb("lg", [128, 4, 8], F32)
        mk = P.sb("mk", [128, 4, 8], F32)
        e1 = P.sb("e1", [128, 4, 8], F32)
        e2 = P.sb("e2", [128, 4, 8], F32)
        m12 = P.sb("m12", [128, 4, 4], F32)
        cmb = P.sb("cmb", [128, 4, 8], F32)
        cT = P.sb("cT", [8, 512], F32)
        gT = comb[:, 0:4, :]
        gk = "comb"
    else:
        gT = P.sb("gT", [128, 4, NP], BF16)
        gk = "gT"

    P.dma(vecs[:], vecs_d, w=["vecs"])
    P.op("dve", lambda e: e.memset(ones[:], 1.0), w=["ones"])
    P.op("dve", lambda e: e.memset(epsc[:], 1e-6), w=["epsc"])
    _stt(P, A2[:], vecs[:, 1, :], 1.0, vecs[:, 0, :], ALU.add, ALU.mult, r=["vecs"], w=["A2"])
    P.dma(wg_sb[:], wglu.rearrange("(kc p) n -> p kc n", p=128), w=["wg"], q="pool")
    if moe:
        P.dma(wr[:], wr_d, w=["wr"])
        P.dma(sel[:], sel_d, w=["sel"])
        P.dma(ident[:], id_d, w=["ident"])
    xv = xT.rearrange("(kc p) t -> p kc t", p=128)
    yv = yT.rearrange("(kc p) t -> p kc t", p=128)
    ov = out.rearrange("(kc p) t -> p kc t", p=128)
    woutv = wout.rearrange("(kc p) n -> p kc n", p=128)
    NG = DFF // 256
    cnt = dict(w=0, h=0, o=0, n=0, s=0, a=0)

    def rms_stats(src_key):
        pass

    for ps_ in range(NTOK // NP):
        t0 = ps_ * NP
        for q in range(4):
            P.dma(x1T[:, q * 4:(q + 1) * 4, :], xv[:, q * 4:(q + 1) * 4, t0:t0 + NP], w=["x1T"], q=("sp" if q % 2 == 0 else "act"))
        P.dma(bufA[:, 4:KC, :], yv[:, 4:KC, t0:t0 + NP], w=["bufA"], q="pool")
        for hf in range(NH):
            tk = slice(hf * 512, (hf + 1) * 512)
            P.dma(ysf[:], yv[:, 0:4, t0 + hf * 512:t0 + (hf + 1) * 512], w=["ysf"])
            _tt(P, ysg[:], ysf[:], ysf[:], ALU.mult, r=["ysf"], w=["ysg"])
            _ts(P, ysg[:], ysg[:], 0.044715, 1.0, ALU.mult, ALU.add, r=["ysg"], w=["ysg"])
            _tt(P, ysg[:], ysg[:], ysf[:], ALU.mult, r=["ysg", "ysf"], w=["ysg"])
            _act(P, ysg[:], ysg[:], AF.Tanh, r=["ysg"], w=["ysg"], scale=float(np.sqrt(2.0 / np.pi)))
            _ts(P, ysg[:], ysg[:], 1.0, 0.5, ALU.add, ALU.mult, r=["ysg"], w=["ysg"])
            _tt(P, gT[:, :, tk], ysg[:], ysf[:], ALU.mult, r=["ysg", "ysf"], w=[gk])
            for oc in range(4):
                k = cnt["n"] % 2
                cnt["n"] += 1
                for kc in range(4):
                    _mm(P, pn[k][:], wg_sb[:, kc, oc * 128:(oc + 1) * 128], gT[:, kc, tk], r=["wg", gk], w=["pn%d" % k],
                        start=(kc == 0), stop=(kc == 3))
                s = cnt["s"] % 2
                cnt["s"] += 1
                _act(P, s_sb[s][:], pn[k][:], AF.Sigmoid, r=["pn%d" % k], w=["s_sb%d" % s])
                _tt(P, bufA[:, oc, tk], s_sb[s][:], gT[:, oc, tk], ALU.mult, r=["s_sb%d" % s, gk], w=["bufA"])
        for og in range(D // 256):
            wi = cnt["w"] % 2
            cnt["w"] += 1
            P.dma(WA[wi][:], woutv[:, :, og * 256:(og + 1) * 256], w=["WA%d" % wi], q="pool")
            for o2 in range(2):
                oc = og * 2 + o2
                for hf in range(NH):
                    tk = slice(hf * 512, (hf + 1) * 512)
                    k = cnt["o"] % 2
                    cnt["o"] += 1
                    for kc in range(KC):
                        _mm(P, po[k][:], WA[wi][:, kc, o2 * 128:(o2 + 1) * 128], bufA[:, kc, tk], r=["WA%d" % wi, "bufA"], w=["po%d" % k],
                            start=(kc == 0), stop=(kc == KC - 1))
                    _stt(P, x1T[:, oc, tk], po[k][:], vecs[:, 3, oc:oc + 1], x1T[:, oc, tk], ALU.mult, ALU.add,
                         r=["po%d" % k, "vecs", "x1T"], w=["x1T"])
        for hf in range(NH):
            tk = slice(hf * 512, (hf + 1) * 512)
            k = cnt["n"] % 2
            cnt["n"] += 1
            for kc in range(KC):
                s = kc % 2
                _act(P, sq[s][:], x1T[:, kc, tk], AF.Square, r=["x1T"], w=["sq%d" % s])
                _mm(P, pn[k][:], ones[:], sq[s][:], r=["ones", "sq%d" % s], w=["pn%d" % k], start=(kc == 0), stop=(kc == KC - 1))
            _act(P, rstd[:], pn[k][:], AF.Sqrt, r=["pn%d" % k, "epsc"], w=["rstd"], scale=1.0 / D, bias=epsc[:, 0:1])
            _rcp(P, rstd[:], rstd[:], r=["rstd"], w=["rstd"])
            k2 = cnt["n"] % 2
            cnt["n"] += 1
            for kc in range(KC):
                s = kc % 2
                _stt(P, sq[s][:], x1T[:, kc, tk], A2[:, kc:kc + 1], rstd[:], ALU.mult, ALU.mult, r=["x1T", "A2", "rstd"], w=["sq%d" % s])
                _act(P, sq[s][:], sq[s][:], AF.Identity, r=["sq%d" % s, "vecs"], w=["sq%d" % s], bias=vecs[:, 2, kc:kc + 1])
                _cp(P, bufA[:, kc, tk], sq[s][:], r=["sq%d" % s], w=["bufA"], eng="dve")
                if moe:
                    _mm(P, pn[k2][0:8, :], wr[:, kc, :], sq[s][:], r=["wr", "sq%d" % s], w=["pn%d" % k2], start=(kc == 0), stop=(kc == KC - 1))
            if moe:
                _cp(P, lgT[:], pn[k2][0:8, :], r=["pn%d" % k2], w=["lgT"])
                k3 = cnt["n"] % 2
                cnt["n"] += 1
                for j in range(4):
                    _tr(P, pn[k3][:, j * 8:(j + 1) * 8], lgT[:, j * 128:(j + 1) * 128], ident[0:8, 0:8], r=["lgT", "ident"], w=["pn%d" % k3])
                _cp(P, lg[:].rearrange("p a b -> p (a b)"), pn[k3][:, 0:32], r=["pn%d" % k3], w=["lg"])
                P.op("dve", lambda e: e.tensor_reduce(out=m12[:, :, 0], in_=lg[:], axis=AX.X, op=ALU.max), r=["lg"], w=["m12"])
                _tt(P, e1[:], lg[:], m12[:, :, 0:1].to_broadcast([128, 4, 8]), ALU.is_equal, r=["lg", "m12"], w=["e1"])
                _stt(P, mk[:], e1[:], -1e30, lg[:], ALU.mult, ALU.add, r=["e1", "lg"], w=["mk"])
                P.op("dve", lambda e: e.tensor_reduce(out=m12[:, :, 1], in_=mk[:], axis=AX.X, op=ALU.max), r=["mk"], w=["m12"])
                _tt(P, e2[:], mk[:], m12[:, :, 1:2].to_broadcast([128, 4, 8]), ALU.is_equal, r=["mk", "m12"], w=["e2"])
                _tt(P, m12[:, :, 2], m12[:, :, 1], m12[:, :, 0], ALU.subtract, r=["m12"], w=["m12"])
                _act(P, m12[:, :, 2], m12[:, :, 2], AF.Exp, r=["m12"], w=["m12"])
                _ts(P, m12[:, :, 2], m12[:, :, 2], 1.0, None, ALU.add, None, r=["m12"], w=["m12"])
                _rcp(P, m12[:, :, 2], m12[:, :, 2], r=["m12"], w=["m12"])
                _ts(P, m12[:, :, 3], m12[:, :, 2], -1.0, 1.0, ALU.mult, ALU.add, r=["m12"], w=["m12"])
                _tt(P, cmb[:], e1[:], m12[:, :, 2:3].to_broadcast([128, 4, 8]), ALU.mult, r=["e1", "m12"], w=["cmb"])
                _tt(P, e2[:], e2[:], m12[:, :, 3:4].to_broadcast([128, 4, 8]), ALU.mult, r=["e2", "m12"], w=["e2"])
                _tt(P, cmb[:], cmb[:], e2[:], ALU.add, r=["cmb", "e2"], w=["cmb"])
                k4 = cnt["n"] % 2
                cnt["n"] += 1
                for j in range(4):
                    _tr(P, pn[k4][0:8, j * 128:(j + 1) * 128], cmb[:, j, :], ident[:], r=["cmb", "ident"], w=["pn%d" % k4])
                _cp(P, cT[:], pn[k4][0:8, :], r=["pn%d" % k4], w=["cT"])
                for ex in range(8):
                    k5 = cnt["n"] % 2
                    cnt["n"] += 1
                    _mm(P, pn[k5][:], sel[:, ex, :], cT[:], r=["sel", "cT"], w=["pn%d" % k5])
                    _cp(P, comb[:, ex, tk], pn[k5][:], r=["pn%d" % k5], w=["comb"], eng=("act" if ex % 2 == 0 else "dve"))
        jobs = [(ex, g) for ex in range(NE) for g in range(NG)]

        def load_w(job, wi):
            ex, g = job
            P.dma(WA[wi][:], w1[ex].rearrange("(kc p) n -> p kc n", p=128)[:, :, g * 256:(g + 1) * 256], w=["WA%d" % wi], q="pool")
            P.dma(WB[wi][:], w3[ex].rearrange("(kc p) n -> p kc n", p=128)[:, :, g * 256:(g + 1) * 256], w=["WB%d" % wi], q="pool")
            P.dma(WC[wi][:], w2[ex, g * 256:(g + 1) * 256, :].rearrange("(c p) n -> p c n", p=128), w=["WC%d" % wi], q="pool")

        wi0 = cnt["w"] % 2
        load_w(jobs[0], wi0)
        for ji, job in enumerate(jobs):
            ex, g = job
            wi = (wi0 + ji) % 2
            if ji + 1 < len(jobs):
                load_w(jobs[ji + 1], (wi + 1) % 2)
            for hf in range(NH):
                tk = slice(hf * 512, (hf + 1) * 512)
                ai = cnt["a"] % 2
                cnt["a"] += 1
                for c in range(2):
                    hk = cnt["h"] % 2
                    cnt["h"] += 1
                    for kc in range(KC):
                        _mm(P, ph1[hk][:], WA[wi][:, kc, c * 128:(c + 1) * 128], bufA[:, kc, tk], r=["WA%d" % wi, "bufA"], w=["ph1_%d" % hk],
                            start=(kc == 0), stop=(kc == KC - 1))
                    for kc in range(KC):
                        _mm(P, ph3[hk][:], WB[wi][:, kc, c * 128:(c + 1) * 128], bufA[:, kc, tk], r=["WB%d" % wi, "bufA"], w=["ph3_%d" % hk],
                            start=(kc == 0), stop=(kc == KC - 1))
                    s = cnt["s"] % 2
                    cnt["s"] += 1
                    _act(P, s_sb[s][:], ph1[hk][:], AF.Silu, r=["ph1_%d" % hk], w=["s_sb%d" % s])
                    _tt(P, a_sb[ai][:, c, :], s_sb[s][:], ph3[hk][:], ALU.mult, r=["s_sb%d" % s, "ph3_%d" % hk], w=["a_sb%d" % ai])
                    if moe:
                        _tt(P, a_sb[ai][:, c, :], a_sb[ai][:, c, :], comb[:, ex, tk], ALU.mult, r=["a_sb%d" % ai, "comb"], w=["a_sb%d" % ai])
                for oc in range(KC):
                    k = cnt["o"] % 2
                    cnt["o"] += 1
                    for c in range(2):
                        _mm(P, po[k][:], WC[wi][:, c, oc * 128:(oc + 1) * 128], a_sb[ai][:, c, :], r=["WC%d" % wi, "a_sb%d" % ai], w=["po%d" % k],
                            start=(c == 0), stop=(c == 1))
                    _stt(P, x1T[:, oc, tk], po[k][:], vecs[:, 4, oc:oc + 1], x1T[:, oc, tk], ALU.mult, ALU.add,
                         r=["po%d" % k, "vecs", "x1T", "x1T%d" % oc], w=["x1T%d" % oc])
        cnt["w"] += len(jobs)
        fin_keys = ["x1T"] + ["x1T%d" % oc for oc in range(KC)]
        P.op("dve", lambda e: e.memset(epsc[:], 1e-6), r=fin_keys, w=["epsc", "x1T"])
        fin_keys = ["x1T"]
        if final:
            for hf in range(NH):
                tk = slice(hf * 512, (hf + 1) * 512)
                k = cnt["n"] % 2
                cnt["n"] += 1
                for kc in range(KC):
                    s = kc % 2
                    _act(P, sq[s][:], x1T[:, kc, tk], AF.Square, r=fin_keys, w=["sq%d" % s])
                    _mm(P, pn[k][:], ones[:], sq[s][:], r=["ones", "sq%d" % s], w=["pn%d" % k], start=(kc == 0), stop=(kc == KC - 1))
                _act(P, rstd[:], pn[k][:], AF.Sqrt, r=["pn%d" % k, "epsc"], w=["rstd"], scale=1.0 / D, bias=epsc[:, 0:1])
                _rcp(P, rstd[:], rstd[:], r=["rstd"], w=["rstd"])
                for kc in range(KC):
                    _stt(P, x1T[:, kc, tk], x1T[:, kc, tk], vecs[:, 5, kc:kc + 1], rstd[:], ALU.mult, ALU.mult, r=fin_keys + ["vecs", "rstd"], w=fin_keys)
        for q in range(4):
            P.dma(ov[:, q * 4:(q + 1) * 4, t0:t0 + NP], x1T[:, q * 4:(q + 1) * 4, :], r=["x1T"], q=("sp" if q % 2 == 0 else "act"))
    P.emit()
    P.close()
    return nc


NCORES = 8
_CORES = list(range(NCORES))


def _run(nc, in_maps):
    res = run_bass_kernel_spmd(nc, in_maps, core_ids=_CORES)
    return res.results


def kernel(x, c, positions, w_ada, b_ada, norm_mix, norm_ffn, norm_final, w_in, w_out,
           ssm_a_re, ssm_a_im, ssm_log_dt, ssm_b_re, ssm_b_im, ssm_c_re, ssm_c_im, ssm_d, ssm_w_glu,
           diff_lam_q1, diff_lam_k1, diff_lam_q2, diff_lam_k2, diff_subln,
           gdn_conv, gdn_a_log, gdn_dt_bias, gdn_norm,
           ffn_w1, ffn_w3, ffn_w2, moe_router, moe_w1, moe_w3, moe_w2):
    f32 = np.float32
    x = np.asarray(x, f32)
    B, S, Dm = x.shape
    HALF = S // 2
    mod = run_k0(np.asarray(c, f32), np.asarray(w_ada, f32), np.asarray(b_ada, f32))
    ident = np.eye(128, dtype=f32)
    invf, sgn, tri = att_consts()
    perm = rope_perm_index()
    msk = gdn_consts()
    sel = np.zeros((8, 8, 128), f32)
    for e in range(8):
        sel[e, e, :] = 1
    xcur = x
    for l in range(2):
        shift1, scale1, gate1, shift2, scale2, gate2 = [mod[l][:, j * 2048:(j + 1) * 2048] for j in range(6)]
        nc = build_ka(HALF, 5900)
        wl = np.ascontiguousarray(w_in[l], dtype=f32)
        in_maps = []
        for i in range(NCORES):
            b, hh = divmod(i, 2)
            vecs = np.ascontiguousarray(np.stack([pscal(norm_mix[l]), pscal(scale1[b]), pscal(shift1[b])], axis=1))
            in_maps.append({"x": np.ascontiguousarray(xcur[b, hh * HALF:(hh + 1) * HALF]), "w": wl, "vecs": vecs, "ident": ident})
        r = _run(nc, in_maps)
        proj = np.empty((B, S, 5900), f32)
        for i in range(NCORES):
            b, hh = divmod(i, 2)
            proj[b, hh * HALF:(hh + 1) * HALF] = r[i]["out"]
        del r
        u = proj[:, :, 0:512]
        dq = proj[:, :, 512:1280].reshape(B, S, 6, 128)
        dk = proj[:, :, 1280:2048].reshape(B, S, 6, 128)
        dv = proj[:, :, 2048:2816].reshape(B, S, 6, 128)
        gqkv = proj[:, :, 2816:5120]
        gz = proj[:, :, 5120:5888]
        ga = proj[:, :, 5888:5894]
        gb = proj[:, :, 5894:5900]
        yT = np.empty((B, Dm, S), f32)
        nc = build_ks5(S)
        in_maps = []
        for i in range(NCORES):
            b, hh = divmod(i, 2)
            g0 = 16 * hh
            lay = s5_layout(ssm_a_re[l], ssm_a_im[l], ssm_log_dt[l], ssm_b_re[l], ssm_b_im[l], ssm_c_re[l], ssm_c_im[l], ssm_d[l], g0)
            lay["uT"] = np.ascontiguousarray(u[b, :, g0 * 16:(g0 + 16) * 16].T)
            in_maps.append(lay)
        r = _run(nc, in_maps)
        for i in range(NCORES):
            b, hh = divmod(i, 2)
            yT[b, 256 * hh:256 * (hh + 1), :] = r[i]["yT"]
        del r
        lam_init = 0.8 - 0.6 * math.exp(-0.3 * l)
        nc = build_katt(S, 3, lam_init)
        lamv = np.ascontiguousarray(np.broadcast_to(np.stack([diff_lam_q1[l], diff_lam_k1[l], diff_lam_q2[l], diff_lam_k2[l]]).astype(f32)[None], (128, 4, 64)))
        subln = np.ascontiguousarray(np.broadcast_to(np.asarray(diff_subln[l], f32)[None], (128, 128)))
        in_maps = []
        for i in range(NCORES):
            b, hh = divmod(i, 2)
            h0 = 3 * hh
            qT = np.ascontiguousarray(dq[b, :, h0:h0 + 3].transpose(1, 2, 0))
            kT = np.ascontiguousarray(dk[b, :, h0:h0 + 3].transpose(1, 2, 0))
            in_maps.append({"qT": qT, "qTp": np.ascontiguousarray(qT[:, perm, :]), "kT": kT, "kTp": np.ascontiguousarray(kT[:, perm, :]),
                            "v": np.ascontiguousarray(dv[b, :, h0:h0 + 3]),
                            "pos": np.ascontiguousarray(np.broadcast_to(np.asarray(positions[b], np.int32)[None, :], (128, S))),
                            "invf": invf, "sgn": sgn, "tri": tri, "lamv": lamv, "subln": subln})
        r = _run(nc, in_maps)
        for i in range(NCORES):
            b, hh = divmod(i, 2)
            yT[b, 512 + 384 * hh:512 + 384 * (hh + 1), :] = r[i]["y"].reshape(S, 384).T
        del r
        nc = build_kgdn(S, 3)
        NCH = S // 64
        in_maps = []
        for i in range(NCORES):
            b, hh = divmod(i, 2)
            h0 = 3 * hh
            q3 = gqkv[b].reshape(S, 3, 6, 128)[:, :, h0:h0 + 3]
            pT = np.ascontiguousarray(q3.transpose(1, 2, 3, 0))
            cw = np.ascontiguousarray(np.asarray(gdn_conv[l], f32).reshape(4, 3, 6, 128)[:, :, h0:h0 + 3].transpose(3, 1, 2, 0))
            zt = np.ascontiguousarray(gz[b].reshape(NCH, 64, 6, 128)[:, :, h0:h0 + 3].transpose(1, 2, 0, 3))
            ab = np.ascontiguousarray(np.stack([ga[b], gb[b]], 0).reshape(2, NCH, 64, 6)[:, :, :, h0:h0 + 3].transpose(2, 0, 3, 1))
            hp = np.ascontiguousarray(np.broadcast_to(np.stack([gdn_a_log[l][h0:h0 + 3], gdn_dt_bias[l][h0:h0 + 3]]).astype(f32)[None], (64, 2, 3)))
            gnw = np.ascontiguousarray(np.broadcast_to(np.asarray(gdn_norm[l], f32)[None], (64, 128)))
            in_maps.append({"pT": pT, "cw": cw, "zt": zt, "ab": ab, "hp": hp, "gnw": gnw, "msk": msk, "ident": ident})
        r = _run(nc, in_maps)
        for i in range(NCORES):
            b, hh = divmod(i, 2)
            yT[b, 1280 + 384 * hh:1280 + 384 * (hh + 1), :] = r[i]["y"].reshape(S, 384).T
        del r, proj
        moe = (l % 2 == 1)
        final = (l == 1)
        if not moe:
            nc = build_kb(HALF, 1024, 5632, 1, moe=False, final=final)
            W1 = np.ascontiguousarray(ffn_w1[l // 2][None], dtype=f32)
            W3 = np.ascontiguousarray(ffn_w3[l // 2][None], dtype=f32)
            W2 = np.ascontiguousarray(ffn_w2[l // 2][None], dtype=f32)
        else:
            nc = build_kb(HALF, 1024, 7168, 8, moe=True, final=final)
            W1 = np.ascontiguousarray(moe_w1[l // 2], dtype=f32)
            W3 = np.ascontiguousarray(moe_w3[l // 2], dtype=f32)
            W2 = np.ascontiguousarray(moe_w2[l // 2], dtype=f32)
        wg = np.ascontiguousarray(ssm_w_glu[l], dtype=f32)
        wo = np.ascontiguousarray(w_out[l], dtype=f32)
        in_maps = []
        for i in range(NCORES):
            b, hh = divmod(i, 2)
            tk = slice(hh * HALF, (hh + 1) * HALF)
            vecs = np.ascontiguousarray(np.stack([pscal(v) for v in (norm_ffn[l], scale2[b], shift2[b], gate1[b], gate2[b], norm_final)], axis=1))
            m = {"xT": np.ascontiguousarray(xcur[b, tk].T), "yT": np.ascontiguousarray(yT[b][:, tk]), "wglu": wg, "wout": wo,
                 "vecs": vecs, "w1": W1, "w3": W3, "w2": W2}
            if moe:
                m["wr"] = np.ascontiguousarray(np.asarray(moe_router[l // 2], f32).reshape(16, 128, 8).transpose(1, 0, 2))
                m["sel"] = sel
                m["ident"] = ident
            in_maps.append(m)
        r = _run(nc, in_maps)
        xnew = np.empty((B, S, Dm), f32)
        for i in range(NCORES):
            b, hh = divmod(i, 2)
            xnew[b, hh * HALF:(hh + 1) * HALF] = r[i]["out"].T
        del r
        xcur = xnew
    return xcur
```
